# Optimizing a Trainium2 kernel written in Bass

```python
import jax, jax.numpy as jnp
from jax import lax
import numpy as np

D_MODEL = 1024
BATCH = 4
SEQ = 4096
DEPTH = 4

HEAD_DIM = 64
N_HEADS_TOTAL = D_MODEL // HEAD_DIM
N_HEADS_FOX = N_HEADS_TOTAL // 2
N_HEADS_SB = N_HEADS_TOTAL - N_HEADS_FOX
N_HEADS_SWA = N_HEADS_TOTAL
N_KV_SWA = max(1, N_HEADS_SWA // 8)
WINDOW = 128
Q_BLOCK = 128
ROPE_THETA = 500000.0
ROPE_DIM = HEAD_DIM // 4
D_FF = ((8 * D_MODEL // 3 + 127) // 128) * 128
N_EXPERTS = 8
TOP_K = 2
D_FF_EXPERT = 7 * D_MODEL // 2
D_PLE = 256
LN_EPS = 1e-5
N_EVEN = (DEPTH + 1) // 2
N_ODD = DEPTH // 2
DEEPNORM_ALPHA = (2.0 * DEPTH) ** 0.25
DEEPNORM_BETA = (8.0 * DEPTH) ** -0.25
FOX_W = N_HEADS_FOX * HEAD_DIM
SB_W = N_HEADS_SB * HEAD_DIM
AB_IN = 3 * FOX_W + N_HEADS_FOX + 3 * SB_W
AB_SPLITS = [FOX_W, 2 * FOX_W, 3 * FOX_W, 3 * FOX_W + N_HEADS_FOX,
             3 * FOX_W + N_HEADS_FOX + SB_W, 3 * FOX_W + N_HEADS_FOX + 2 * SB_W]
SWA_IN = (N_HEADS_SWA + 2 * N_KV_SWA) * HEAD_DIM
SWA_SPLITS = [N_HEADS_SWA * HEAD_DIM, (N_HEADS_SWA + N_KV_SWA) * HEAD_DIM]

kernel_name = 'fox_stickbreak_swa_sink_moe_deepnorm_ple'

F32 = jnp.float32


def layer_norm(x, g, b):
    xf = x.astype(F32)
    mu = jnp.mean(xf, axis=-1, keepdims=True)
    xc = xf - mu
    var = jnp.mean(xc * xc, axis=-1, keepdims=True)
    return (xc * lax.rsqrt(var + LN_EPS) * g.astype(F32) + b.astype(F32)).astype(x.dtype)


def swiglu(x, w_gate_up, w_down):
    g, u = jnp.split(x @ w_gate_up, 2, axis=-1)
    return (jax.nn.silu(g) * u) @ w_down


def partial_rope(x, pos):
    half = ROPE_DIM // 2
    inv = ROPE_THETA ** (-jnp.arange(half, dtype=F32) * 2.0 / ROPE_DIM)
    ang = pos.astype(F32)[:, None] * inv[None, :]
    cos = jnp.cos(ang)[None, :, None, :]
    sin = jnp.sin(ang)[None, :, None, :]
    xr = x[..., :ROPE_DIM].astype(F32)
    x1, x2 = xr[..., :half], xr[..., half:]
    rot = jnp.concatenate([x1 * cos - x2 * sin, x2 * cos + x1 * sin], axis=-1).astype(x.dtype)
    return jnp.concatenate([rot, x[..., ROPE_DIM:]], axis=-1)


def fox_attention(q, k, v, log_f):
    B, S, H, dh = q.shape
    scale = dh ** -0.5
    c = jnp.cumsum(log_f, axis=1).transpose(0, 2, 1)
    k_pos = jnp.arange(S)

    def block(i):
        start = i * Q_BLOCK
        qb = lax.dynamic_slice_in_dim(q, start, Q_BLOCK, axis=1)
        cb = lax.dynamic_slice_in_dim(c, start, Q_BLOCK, axis=2)
        s = jnp.einsum('bqhd,bkhd->bhqk', qb, k, preferred_element_type=F32) * scale
        s = s + cb[..., :, None] - c[..., None, :]
        q_pos = start + jnp.arange(Q_BLOCK)
        mask = k_pos[None, :] <= q_pos[:, None]
        w = jax.nn.softmax(jnp.where(mask, s, -jnp.inf), axis=-1)
        return jnp.einsum('bhqk,bkhd->bqhd', w.astype(v.dtype), v)

    out = lax.map(block, jnp.arange(S // Q_BLOCK))
    return out.transpose(1, 0, 2, 3, 4).reshape(B, S, H, dh)


def stick_breaking_attention(q, k, v):
    B, S, H, dh = q.shape
    scale = dh ** -0.5
    k_pos = jnp.arange(S)

    def block(i):
        start = i * Q_BLOCK
        qb = lax.dynamic_slice_in_dim(q, start, Q_BLOCK, axis=1)
        z = jnp.einsum('bqhd,bkhd->bhqk', qb, k, preferred_element_type=F32) * scale
        q_pos = start + jnp.arange(Q_BLOCK)
        mask = k_pos[None, :] < q_pos[:, None]
        log_1mb = jnp.where(mask, jax.nn.log_sigmoid(-z), 0.0)
        later = lax.cumsum(log_1mb, axis=3, reverse=True) - log_1mb
        a = jnp.where(mask, jnp.exp(jax.nn.log_sigmoid(z) + later), 0.0)
        return jnp.einsum('bhqk,bkhd->bqhd', a.astype(v.dtype), v)

    out = lax.map(block, jnp.arange(S // Q_BLOCK))
    return out.transpose(1, 0, 2, 3, 4).reshape(B, S, H, dh)


def sliding_window_gqa_sinks(q, k, v, sinks):
    B, S, HQ, dh = q.shape
    HKV = k.shape[2]
    G = HQ // HKV
    W = WINDOW
    n = S // W
    scale = dh ** -0.5
    qb = q.reshape(B, n, W, HKV, G, dh)

    def with_prev(t):
        t = t.reshape(B, n, W, HKV, dh)
        prev = jnp.concatenate([jnp.zeros_like(t[:, :1]), t[:, :-1]], axis=1)
        return jnp.concatenate([prev, t], axis=2)

    kk, vv = with_prev(k), with_prev(v)
    s = jnp.einsum('bnqhgd,bnkhd->bnhgqk', qb, kk, preferred_element_type=F32) * scale
    diff = jnp.arange(W)[:, None] + W - jnp.arange(2 * W)[None, :]
    band = (diff >= 0) & (diff < W)
    key_abs = jnp.arange(n)[:, None] * W + jnp.arange(2 * W)[None, :] - W
    mask = band[None] & (key_abs >= 0)[:, None, :]
    s = jnp.where(mask[None, :, None, None], s, -jnp.inf)
    sink = sinks.astype(F32).reshape(HKV, G)[None, None, :, :, None]
    mx = jnp.maximum(jnp.max(s, axis=-1), sink)
    e = jnp.exp(s - mx[..., None])
    w = e / (jnp.sum(e, axis=-1) + jnp.exp(sink - mx))[..., None]
    out = jnp.einsum('bnhgqk,bnkhd->bnqhgd', w.astype(v.dtype), vv)
    return out.reshape(B, S, HQ, dh)


def fox_sb_mixer(x, w_in, b_f, w_out):
    B, S, _ = x.shape
    h = x @ w_in
    qa, ka, va, fa, qs, ks, vs = jnp.split(h, AB_SPLITS, axis=-1)
    hd = lambda t, H: t.reshape(B, S, H, HEAD_DIM)
    log_f = jax.nn.log_sigmoid((fa + b_f).astype(F32))
    oa = fox_attention(hd(qa, N_HEADS_FOX), hd(ka, N_HEADS_FOX), hd(va, N_HEADS_FOX), log_f)
    ob = stick_breaking_attention(hd(qs, N_HEADS_SB), hd(ks, N_HEADS_SB), hd(vs, N_HEADS_SB))
    o = jnp.concatenate([oa.reshape(B, S, FOX_W), ob.reshape(B, S, SB_W)], axis=-1)
    return o @ w_out


def swa_mixer(x, w_qkv, sinks, w_out, pos):
    B, S, _ = x.shape
    q, k, v = jnp.split(x @ w_qkv, SWA_SPLITS, axis=-1)
    q = partial_rope(q.reshape(B, S, N_HEADS_SWA, HEAD_DIM), pos)
    k = partial_rope(k.reshape(B, S, N_KV_SWA, HEAD_DIM), pos)
    v = v.reshape(B, S, N_KV_SWA, HEAD_DIM)
    o = sliding_window_gqa_sinks(q, k, v, sinks)
    return o.reshape(B, S, N_HEADS_SWA * HEAD_DIM) @ w_out


def moe_swiglu(x, router_w, router_b, w_gate_up, w_down):
    B, S, D = x.shape
    xf = x.reshape(-1, D)
    logits = jnp.einsum('td,de->te', xf, router_w, preferred_element_type=F32) + router_b.astype(F32)
    top_v, top_i = lax.top_k(logits, TOP_K)
    gates = jax.nn.softmax(top_v, axis=-1)
    combine = jnp.sum(gates[..., None] * jax.nn.one_hot(top_i, N_EXPERTS, dtype=F32), axis=1)
    y = jnp.zeros(xf.shape, F32)
    for e in range(N_EXPERTS):
        y = y + combine[:, e:e + 1] * swiglu(xf, w_gate_up[e], w_down[e]).astype(F32)
    return y.astype(x.dtype).reshape(B, S, D)


def setup_inputs(seed: int = 0) -> dict:
    key = jax.random.key(seed)
    ks = jax.random.split(key, 20)
    nrm = lambda k, shape, s: jax.random.normal(k, shape, F32) * s
    return {
        'x': nrm(ks[0], (BATCH, SEQ, D_MODEL), 1.0),
        'p': nrm(ks[1], (DEPTH, BATCH, SEQ, D_PLE), 1.0),
        'ln_mix_g': 1.0 + nrm(ks[2], (DEPTH, D_MODEL), 0.02),
        'ln_mix_b': nrm(ks[3], (DEPTH, D_MODEL), 0.02),
        'ln_ffn_g': 1.0 + nrm(ks[4], (DEPTH, D_MODEL), 0.02),
        'ln_ffn_b': nrm(ks[5], (DEPTH, D_MODEL), 0.02),
        'ab_w_in': nrm(ks[6], (N_EVEN, D_MODEL, AB_IN), D_MODEL ** -0.5),
        'ab_b_f': jax.random.uniform(ks[7], (N_EVEN, N_HEADS_FOX), F32, 1.0, 5.0),
        'ab_w_out': nrm(ks[8], (N_EVEN, FOX_W + SB_W, D_MODEL), (FOX_W + SB_W) ** -0.5 * DEEPNORM_BETA),
        'c_w_qkv': nrm(ks[9], (N_ODD, D_MODEL, SWA_IN), D_MODEL ** -0.5),
        'c_sinks': nrm(ks[10], (N_ODD, N_HEADS_SWA), 1.0),
        'c_w_out': nrm(ks[11], (N_ODD, N_HEADS_SWA * HEAD_DIM, D_MODEL), (N_HEADS_SWA * HEAD_DIM) ** -0.5 * DEEPNORM_BETA),
        'ffn_w_gate_up': nrm(ks[12], (N_EVEN, D_MODEL, 2 * D_FF), D_MODEL ** -0.5),
        'ffn_w_down': nrm(ks[13], (N_EVEN, D_FF, D_MODEL), D_FF ** -0.5 * DEEPNORM_BETA),
        'router_w': nrm(ks[14], (N_ODD, D_MODEL, N_EXPERTS), D_MODEL ** -0.5),
        'router_b': nrm(ks[15], (N_ODD, N_EXPERTS), 0.01),
        'moe_w_gate_up': nrm(ks[16], (N_ODD, N_EXPERTS, D_MODEL, 2 * D_FF_EXPERT), D_MODEL ** -0.5),
        'moe_w_down': nrm(ks[17], (N_ODD, N_EXPERTS, D_FF_EXPERT, D_MODEL), D_FF_EXPERT ** -0.5 * DEEPNORM_BETA),
        'ple_w_gate': nrm(ks[18], (DEPTH, D_MODEL, D_MODEL), D_MODEL ** -0.5),
        'ple_w_proj': nrm(ks[19], (DEPTH, D_PLE, D_MODEL), D_PLE ** -0.5 * 0.5),
    }


def reference(x, p, ln_mix_g, ln_mix_b, ln_ffn_g, ln_ffn_b, ab_w_in, ab_b_f, ab_w_out,
              c_w_qkv, c_sinks, c_w_out, ffn_w_gate_up, ffn_w_down, router_w, router_b,
              moe_w_gate_up, moe_w_down, ple_w_gate, ple_w_proj):
    pos = jnp.arange(x.shape[1])
    for i in range(DEPTH):
        j = i // 2
        if i % 2 == 0:
            h = fox_sb_mixer(x, ab_w_in[j], ab_b_f[j], ab_w_out[j])
        else:
            h = swa_mixer(x, c_w_qkv[j], c_sinks[j], c_w_out[j], pos)
        x = layer_norm(DEEPNORM_ALPHA * x + h, ln_mix_g[i], ln_mix_b[i])
        if i % 2 == 0:
            h = swiglu(x, ffn_w_gate_up[j], ffn_w_down[j])
        else:
            h = moe_swiglu(x, router_w[j], router_b[j], moe_w_gate_up[j], moe_w_down[j])
        x = layer_norm(DEEPNORM_ALPHA * x + h, ln_ffn_g[i], ln_ffn_b[i])
        x = x + jax.nn.sigmoid(x @ ple_w_gate[i]) * (p[i] @ ple_w_proj[i])
    return x
```

```python
import os
import numpy as np
import concourse.bass as bass
import concourse.mybir as mybir
from concourse.bass_utils import run_bass_kernel_spmd

F32 = mybir.dt.float32
BF16 = mybir.dt.bfloat16
AF = mybir.ActivationFunctionType
ALU = mybir.AluOpType
AX = mybir.AxisListType

D = 1024
S = 4096
DEPTH = 4
T = 2048
NT = 16
ALPHA = (2.0 * DEPTH) ** 0.25
EPS = 1e-5
OWN = [[0, 3, 4, 7], [1, 2, 5, 6]]
OWNER = [0, 1, 1, 0, 0, 1, 1, 0]
LIDX = [0, 0, 1, 1, 2, 2, 3, 3]
GSMIN = [0, 2, 4, 6]
NEG = -30000.0
D_FF = 2816
D_FFE = 3584
EXR = 2072
SWA_CANDS = [[(0, 0)], [(1, 1), (1, 0)], [(0, 1), (0, 2)], [(1, 3), (1, 2)]]


class Buf:
    __slots__ = ("name", "w", "r", "dsem", "dcnt", "persist", "kind")

    def __init__(self, name, persist=False):
        self.name = name
        self.w = None
        self.r = {}
        self.dsem = None
        self.dcnt = 0
        self.persist = persist
        self.kind = None


class Eng:
    def __init__(self, K, name, e, is_pe=False):
        self.K = K
        self.name = name
        self.e = e
        self.sem = K.nc.alloc_semaphore("es_" + name)
        self.count = 0
        self.waited = {}
        self.is_pe = is_pe

    def wait(self, ev):
        sem, val = ev
        if self.is_pe and sem is self.sem:
            return
        owner = self.K.sem_owner.get(sem)
        if owner is not None:
            val = max(val, 16 * owner.dcnt)
        if self.waited.get(sem, 0) >= val:
            return
        self.waited[sem] = val
        self.e.wait_ge(sem, val)

    def op(self, fn, R=(), W=(), signal=True):
        for b in R:
            if b.w is not None:
                self.wait(b.w)
        for b in W:
            if b.w is not None:
                self.wait(b.w)
            for ev in b.r.values():
                self.wait(ev)
        ins = fn(self.e)
        if signal:
            self.count += 1
            ins.then_inc(self.sem, 1)
            ev = (self.sem, self.count)
        else:
            ev = (self.sem, self.count + 1)
        for b in R:
            b.r[self.name] = ev
        for b in W:
            b.w = ev
            b.r = {}
        return ins

    def dma(self, out, in_, W, R=(), **kw):
        K = self.K
        kind = "sw" if self is K.pool else "hw"
        if W.kind is None:
            W.kind = kind
        assert W.kind == kind, (W.name, W.kind, kind)
        if W.dsem is None:
            if K.sem_pool[kind] and not W.persist:
                W.dsem, W.dcnt = K.sem_pool[kind].pop()
            else:
                W.dsem = K.nc.alloc_semaphore("ds_%d" % K.nsem)
                K.nsem += 1
            K.dbufs.append(W)
            K.sem_owner[W.dsem] = W
        for b in R:
            if b.w is not None:
                self.wait(b.w)
        same = (W.w is not None and W.w[0] is W.dsem and not W.r)
        if not same:
            if W.w is not None:
                self.wait(W.w)
            for ev in W.r.values():
                self.wait(ev)
        W.dcnt += 1
        self.e.dma_start(out=out, in_=in_, **kw).then_inc(W.dsem, 16)
        ev = (W.dsem, 16 * W.dcnt)
        for b in R:
            b.r[("d", W.dsem)] = ev
        W.w = ev
        W.r = {}


class Kern:
    def __init__(self, nc):
        self.nc = nc
        self.dbufs = []
        self.sem_pool = {"sw": [], "hw": []}
        self.sem_owner = {}
        self.nsem = 0
        self.pe = Eng(self, "pe", nc.tensor, is_pe=True)
        self.act = Eng(self, "act", nc.scalar)
        self.dve = Eng(self, "dve", nc.vector)
        self.pool = Eng(self, "pool", nc.gpsimd)
        self.sp = Eng(self, "sp", nc.sync)
        self.engs = [self.pe, self.act, self.dve, self.pool, self.sp]
        self.extra_evs = []

    def barrier(self):
        evs = [(e.sem, e.count) for e in self.engs if e.count > 0]
        evs += [(b.dsem, 16 * b.dcnt) for b in self.dbufs if b.dcnt > 0]
        evs += self.extra_evs
        for e in self.engs:
            for ev in evs:
                if ev[0] is e.sem:
                    continue
                e.wait(ev)
        keep = []
        for b in self.dbufs:
            if b.persist:
                keep.append(b)
            else:
                self.sem_pool[b.kind].append((b.dsem, b.dcnt))
                b.dsem = None
        self.dbufs = keep


def bufs(prefix, n, persist=False):
    return [Buf("%s%d" % (prefix, i), persist) for i in range(n)]


def build(n_layers=DEPTH, dbg_stage=None, layer0=0):
    from contextlib import ExitStack
    nc = bass.Bass("TRN2", target_bir_lowering=False)
    K = Kern(nc)
    pe, act, dve, pool, sp = K.pe, K.act, K.dve, K.pool, K.sp

    def din(name, shape, dt=F32):
        return nc.dram_tensor(name, list(shape), dt, kind="ExternalInput").ap()

    x_in = din("x_own", [T, D])
    p_in = din("p_own", [DEPTH, T, 256])
    lng_in = din("ln_g", [2 * DEPTH, D])
    lnb_in = din("ln_b", [2 * DEPTH, D])
    ab_w_in = din("ab_w_in", [2, D, 3080])
    wfT_in = din("ab_wfT", [2, 8, D])
    ab_bf_in = din("ab_b_f", [2, 8])
    ab_w_out = din("ab_w_out", [2, D, D])
    c_w_qkv = din("c_w_qkv", [2, D, 1280])
    c_sinks = din("c_sinks", [2, 16])
    c_w_out = din("c_w_out", [2, D, D])
    ffn_gu = din("ffn_w_gate_up", [2, D, 2 * D_FF])
    ffn_dn = din("ffn_w_down", [2, D_FF, D])
    rT_in = din("routerT", [2, 8, D])
    router_in = din("router_w", [2, D, 8])
    rb_in = din("router_b", [2, 8])
    moe_gu = din("moe_w_gate_up", [2, 8, D, 2 * D_FFE])
    moe_dn = din("moe_w_down", [2, 8, D_FFE, D])
    ple_g = din("ple_w_gate", [DEPTH, D, D])
    ple_p = din("ple_w_proj", [DEPTH, 256, D])
    mle_in = din("mask_le", [2, 8, 128, 512])
    mlt_in = din("mask_lt", [2, 8, 128, 512])
    osel_in = din("osel", [1, 8])
    msw_in = din("mask_swa", [4, 6, 128, 512])
    cos_in = din("rope_cos", [T, 8])
    sin_in = din("rope_sin", [T, 8])
    id_in = din("ident", [128, 128])
    triu_in = din("triu_neg", [128, 128])
    out = nc.dram_tensor("out", [T, D], F32, kind="ExternalOutput").ap()

    EXC = [512, 512, 512, 512, 24]
    ex_mine_c = [nc.dram_tensor("ex_mine%d" % i, [n, T], BF16) for i, n in enumerate(EXC)]
    ex_all_c = [nc.dram_tensor("ex_all%d" % i, [2 * n, T], BF16) for i, n in enumerate(EXC)]

    def exm(r0, r1):
        i = r0 // 512
        assert (r1 - 1) // 512 == i
        return ex_mine_c[i].ap()[r0 - i * 512:r1 - i * 512, :]

    def exa(rank, r0, r1):
        i = r0 // 512
        assert (r1 - 1) // 512 == i
        b = rank * EXC[i] - i * 512
        return ex_all_c[i].ap()[b + r0:b + r1, :]

    def tokview(ap2d):
        return ap2d.rearrange("r (a c) -> (r a) c", a=2)
    ex2_mine = nc.dram_tensor("ex2_mine", [256, T], BF16)
    ex2_all = nc.dram_tensor("ex2_all", [512, T], BF16)
    q_scr = nc.dram_tensor("q_scr", [16, 64, T], BF16)
    c_scr = nc.dram_tensor("c_scr", [8, 6, S], BF16)
    cq_scr = nc.dram_tensor("cq_scr", [8, 3, T], BF16)
    ot_scr = nc.dram_tensor("ot_scr", [16, 64, T], BF16)
    B_exm, B_exa, B_ex2m, B_ex2a = Buf("exm", True), Buf("exa", True), Buf("ex2m", True), Buf("ex2a", True)
    B_qscr, B_cscr, B_otscr, B_out, B_cqscr = Buf("qscr", True), Buf("cscr", True), Buf("otscr", True), Buf("out", True), Buf("cqscr", True)
    cc_sem = nc.alloc_semaphore("cc_sem")
    cc_cnt = [0]

    def allgather(src, dst, Bsrc, Bdst):
        if Bsrc.w is not None:
            pool.wait(Bsrc.w)
        if Bdst.w is not None:
            pool.wait(Bdst.w)
        for ev in Bdst.r.values():
            pool.wait(ev)
        srcs = src if isinstance(src, list) else [src]
        dsts = dst if isinstance(dst, list) else [dst]
        for s_, d_ in zip(srcs, dsts):
            cc_cnt[0] += 1
            pool.e.collective_compute(
                "AllGather", ALU.bypass,
                replica_groups=[[0, 1], [2, 3], [4, 5], [6, 7]],
                ins=[s_.ap().opt()], outs=[d_.ap().opt()],
            ).then_inc(cc_sem, 1)
        ev = (cc_sem, cc_cnt[0])
        K.extra_evs.append(ev)
        Bsrc.r["cc"] = ev
        Bdst.w = ev
        Bdst.r = {}

    X = nc.alloc_sbuf_tensor("X", [128, NT, D], F32)
    XT = nc.alloc_sbuf_tensor("XT", [128, 8, T], BF16)
    ident_b = nc.alloc_sbuf_tensor("ident_b", [128, 128], BF16)
    ident_f = nc.alloc_sbuf_tensor("ident_f", [128, 128], F32)
    triu = nc.alloc_sbuf_tensor("triu", [128, 128], BF16)
    negones = nc.alloc_sbuf_tensor("negones", [128, 128], BF16)
    ones_f = nc.alloc_sbuf_tensor("ones_f", [128, 64], F32)
    BX = bufs("X", NT, True)
    BXT = bufs("XT", NT, True)
    Bc = Buf("consts", True)
    BXld = Buf("xld", True)

    pool.dma(ident_b[:], id_in[:, :], Bc)
    pool.dma(ident_f[:], id_in[:, :], Bc)
    pool.dma(triu[:], triu_in[:, :], Bc)
    dve.op(lambda e: e.memset(negones[:], -1.0), W=[Bc])
    dve.op(lambda e: e.memset(ones_f[:], 1.0), W=[Bc])
    for t in range(NT):
        sp.dma(X[:, t, :], x_in[t * 128:(t + 1) * 128, :], BXld)
    for t in range(NT):
        BX[t].w = BXld.w

    def make_xt(ps_tr, Bps, xb, Bxb):
        for t in range(NT):
            s = t % 2
            act.op(lambda e: e.copy(xb[:, s, :], X[:, t, :]), R=[BX[t]], W=[Bxb[s]])
            for k in range(8):
                pe.op(lambda e: e.transpose(ps_tr[:, s, k * 128:(k + 1) * 128], xb[:, s, k * 128:(k + 1) * 128], ident_b[:]),
                      R=[Bxb[s], Bc], W=[Bps[s]], signal=(k == 7))
            dve.op(lambda e: e.tensor_copy(XT[:, :, t * 128:(t + 1) * 128],
                                           ps_tr[:, s, :].rearrange("p (k n) -> p k n", k=8)),
                   R=[Bps[s]], W=[BXT[t]])

    def layer_norm_tiles(idx, get_y, gb, Bgb, tmp, Btmp, st, Bst):
        sp.dma(gb[:, 0, :], lng_in[idx:idx + 1, :].partition_broadcast(128), Bgb)
        sp.dma(gb[:, 1, :], lnb_in[idx:idx + 1, :].partition_broadcast(128), Bgb)
        for t in range(NT):
            s = t % 2
            get_y(t, s)
            for c in range(2):
                dve.op(lambda e: e.bn_stats(st[:, s, c * 6:(c + 1) * 6], tmp[:, s, c * 512:(c + 1) * 512]),
                       R=[Btmp[s]], W=[Bst[s]])
            dve.op(lambda e: e.bn_aggr(st[:, s, 12:14], st[:, s, 0:12]), R=[Bst[s]], W=[Bst[s]])
            dve.op(lambda e: e.tensor_scalar_add(st[:, s, 14:15], st[:, s, 13:14], EPS), R=[Bst[s]], W=[Bst[s]])
            act.op(lambda e: e.sqrt(st[:, s, 15:16], st[:, s, 14:15]), R=[Bst[s]], W=[Bst[s]])
            dve.op(lambda e: e.reciprocal(st[:, s, 16:17], st[:, s, 15:16]), R=[Bst[s]], W=[Bst[s]])
            dve.op(lambda e: e.tensor_scalar(tmp[:, s, :], tmp[:, s, :], st[:, s, 12:13], st[:, s, 16:17],
                                             ALU.subtract, ALU.mult), R=[Btmp[s], Bst[s]], W=[Btmp[s]])
            pool.op(lambda e: e.tensor_mul(tmp[:, s, :], tmp[:, s, :], gb[:, 0, :]), R=[Btmp[s], Bgb], W=[Btmp[s]])
            pool.op(lambda e: e.tensor_add(X[:, t, :], tmp[:, s, :], gb[:, 1, :]), R=[Btmp[s], Bgb], W=[BX[t]])

    o_ctr = [0]

    def attention_core(kind, qt, Bqt, kt, Bkt, vt, Bvt, blocks_for_ls, ps, Bps_, sb, Bsb, sink_ap=None, Bsink=None):
        S_ps, O_ps2, T_ps, R_ps, Bc_ps = ps["S"], ps["O"], ps["T"], ps["R"], ps["B"]
        BS, BO2, BT, BR, BB = Bps_["S"], Bps_["O"], Bps_["T"], Bps_["R"], Bps_["B"]
        at, Bat = sb["at"], Bsb["at"]
        ot, Bot = sb["ot"], Bsb["ot"]
        el, ll = sb["el"], sb["ll"]
        Bel, Bll = Bsb["el"], Bsb["ll"]
        rs, Brs = sb["rs"], Bsb["rs"]
        for ls in range(4):
            o_ctr[0] += 1
            O_ps = O_ps2[:, o_ctr[0] % 2, :]
            BO = BO2[o_ctr[0] % 2]
            qs = qt[:, ls * 512:(ls + 1) * 512]
            blks = blocks_for_ls(ls)
            n = len(blks)
            if kind == "sb":
                blks = blks[::-1]

            sm_slots = [(S_ps[:, 0, :], BS[0]), (S_ps[:, 1, :], BS[1]), (T_ps[:, 0, :], BT[0]), (T_ps[:, 1, :], BT[1])]

            def scores(i, slot, close):
                kc, vb, m_ap, Bm = blks[i]
                dst, Bd = slot
                ksl = kt[:, kc:kc + 128]
                last = close and (m_ap is None)
                pe.op(lambda e: e.matmul(dst, ksl, qs, start=True, stop=last),
                      R=[Bkt, Bqt], W=[Bd], signal=last)
                if m_ap is not None:
                    pe.op(lambda e: e.matmul(dst, ident_b[:], m_ap, start=False, stop=close),
                          R=[Bm, Bc], W=[Bd], signal=close)

            def pv(i, nslot):
                kc, vb, m_ap, Bm = blks[i]
                s = i % nslot
                pe.op(lambda e: e.matmul(O_ps[0:65, :], vt[:, vb, :], at[:, s, :], start=(i == 0), stop=(i == n - 1)),
                      R=[Bvt, Bat[s]], W=[BO], signal=True)

            if kind == "sm":
                LA = 3
                for i in range(min(LA, n)):
                    scores(i, sm_slots[i % 4], True)
                for i in range(n):
                    s = i % 4
                    if i + LA < n:
                        scores(i + LA, sm_slots[(i + LA) % 4], True)
                    act.op(lambda e: e.activation(at[:, s, :], sm_slots[s][0], AF.Exp), R=[sm_slots[s][1]], W=[Bat[s]])
                    if i >= 1:
                        pv(i - 1, 4)
                pv(n - 1, 4)
            else:
                def stage_b(i):
                    s = i % 2
                    act.op(lambda e: e.activation(el[:, s, :], S_ps[:, s, :], AF.Exp), R=[BS[s]], W=[Bel[s]])
                    act.op(lambda e: e.activation(ll[:, s, :], el[:, s, :], AF.Ln, bias=1.0), R=[Bel[s]], W=[Bll[s]])
                scores(0, (S_ps[:, 0, :], BS[0]), True)
                stage_b(0)
                for i in range(n):
                    s = i % 2
                    if i + 1 < n:
                        scores(i + 1, (S_ps[:, (i + 1) % 2, :], BS[(i + 1) % 2]), True)
                        stage_b(i + 1)
                    scores(i, (T_ps[:, s, :], BT[s]), False)
                    pe.op(lambda e: e.matmul(T_ps[:, s, :], triu[:], ll[:, s, :], start=False, stop=True),
                          R=[Bll[s], Bc], W=[BT[s]])
                    if i < n - 1:
                        pe.op(lambda e: e.matmul(R_ps[:, :], negones[:], ll[:, s, :], start=True, stop=True),
                              R=[Bll[s], Bc], W=[BR], signal=True)
                    if i == 0:
                        act.op(lambda e: e.activation(at[:, s, :], T_ps[:, s, :], AF.Exp), R=[BT[s]], W=[Bat[s]])
                    else:
                        dve.op(lambda e: e.tensor_tensor(el[:, s, :], T_ps[:, s, :], rs[:, :], ALU.add),
                               R=[BT[s], Brs], W=[Bel[s]])
                        act.op(lambda e: e.activation(at[:, s, :], el[:, s, :], AF.Exp), R=[Bel[s]], W=[Bat[s]])
                    if i < n - 1:
                        if i == 0:
                            dve.op(lambda e: e.tensor_copy(rs[:, :], R_ps[:, :]), R=[BR], W=[Brs])
                        else:
                            dve.op(lambda e: e.tensor_tensor(rs[:, :], rs[:, :], R_ps[:, :], ALU.add), R=[BR, Brs], W=[Brs])
                    if i >= 1:
                        pv(i - 1, 2)
                pv(n - 1, 2)
            osl = ot[0:64, ls * 512:(ls + 1) * 512]
            if kind == "sm":
                rd = sb["rd"]
                Brd = Bsb["rd"]
                if sink_ap is not None:
                    dve.op(lambda e: e.tensor_scalar_add(rd[64:65, :], O_ps[64:65, :], sink_ap), R=[BO, Bsink], W=[Brd])
                    dve.op(lambda e: e.reciprocal(rd[64:65, :], rd[64:65, :]), R=[Brd], W=[Brd])
                else:
                    dve.op(lambda e: e.reciprocal(rd[64:65, :], O_ps[64:65, :]), R=[BO], W=[Brd])
                pe.op(lambda e: e.matmul(Bc_ps[0:64, :], ones_f[64:65, 0:64], rd[64:65, :], start=True, stop=True),
                      R=[Brd, Bc], W=[BB])
                act.op(lambda e: e.copy(sb["bc"][0:64, :], Bc_ps[0:64, :]), R=[BB], W=[Bsb["bc"]])
                dve.op(lambda e: e.tensor_tensor(osl, O_ps[0:64, :], sb["bc"][0:64, :], ALU.mult),
                       R=[BO, Bsb["bc"]], W=[Bot])
            else:
                dve.op(lambda e: e.tensor_copy(osl, O_ps[0:64, :]), R=[BO], W=[Bot])

    from contextlib import ExitStack

    uid = [0]

    def SB(es, name, shape, dt):
        uid[0] += 1
        return es.enter_context(nc.sbuf_tensor("%s_%d" % (name, uid[0]), list(shape), dt))

    def PS(es, name, shape, dt):
        uid[0] += 1
        return es.enter_context(nc.psum_tensor("%s_%d" % (name, uid[0]), list(shape), dt))

    def split3(es, tag, src, Bsrc, dst3, Bdst, width, neg_dst=None):
        r = SB(es, "sp_r" + tag, [8, width], F32)
        Br = Buf("spr")
        dve.op(lambda e: e.tensor_copy(dst3[:, 0, :], src), R=[Bsrc], W=[Bdst])
        dve.op(lambda e: e.tensor_tensor(r[:], src, dst3[:, 0, :], ALU.subtract), R=[Bsrc, Bdst], W=[Br])
        dve.op(lambda e: e.tensor_copy(dst3[:, 1, :], r[:]), R=[Br], W=[Bdst])
        dve.op(lambda e: e.tensor_tensor(r[:], r[:], dst3[:, 1, :], ALU.subtract), R=[Br, Bdst], W=[Br])
        dve.op(lambda e: e.tensor_copy(dst3[:, 2, :], r[:]), R=[Br], W=[Bdst])
        if neg_dst is not None:
            for j in range(3):
                dve.op(lambda e: e.tensor_scalar_mul(neg_dst[:, j, :], dst3[:, j, :], -1.0), R=[Bdst], W=[Bdst])

    def fp32_proj8(es, w_ap, out_sb, Bout):
        wsb = SB(es, "w8", [128, 8, 8], F32)
        xtf = SB(es, "xtf", [128, 2, D], F32)
        XTp = PS(es, "XTp", [128, 2, 1024], F32)
        FAp = PS(es, "FAp", [128, NT, 8], F32)
        Bw, Bxtf, BXTp, BFAp = Buf("w8"), bufs("xtf", 2), bufs("XTp", 2), Buf("FAp")
        sp.dma(wsb[:], w_ap.rearrange("(k p) e -> p k e", p=128), Bw)
        for t in range(NT):
            s = t % 2
            for k in range(8):
                pe.op(lambda e: e.transpose(XTp[:, s, k * 128:(k + 1) * 128], X[:, t, k * 128:(k + 1) * 128], ident_f[:]),
                      R=[BX[t], Bc], W=[BXTp[s]], signal=(k == 7))
            dve.op(lambda e: e.tensor_copy(xtf[:, s, 0:512], XTp[:, s, 0:512]), R=[BXTp[s]], W=[Bxtf[s]])
            act.op(lambda e: e.copy(xtf[:, s, 512:1024], XTp[:, s, 512:1024]), R=[BXTp[s]], W=[Bxtf[s]])
            for k in range(8):
                pe.op(lambda e: e.matmul(FAp[:, t, :], xtf[:, s, k * 128:(k + 1) * 128], wsb[:, k, :], start=(k == 0), stop=(k == 7)),
                      R=[Bxtf[s], Bw], W=[BFAp], signal=(k == 7))
        dve.op(lambda e: e.tensor_copy(out_sb[:, :, :], FAp[:, :, :]), R=[BFAp], W=[Bout])

    def even_proj(j):
        with ExitStack() as es:
            bfb = SB(es, "bfb", [128, 8], F32)
            fa = SB(es, "fa", [128, NT, 8], F32)
            lfT = SB(es, "lfT", [8, T], F32)
            lf3 = SB(es, "lf3", [8, 3, T], BF16)
            P = PS(es, "Pl", [128, 2, 512], F32)
            Bwfb, Bbfb, Bjunk, Bfa, BlfT, Blf3 = Buf("wfb"), Buf("bfb"), bufs("junk", 2), Buf("fa"), Buf("lfT"), Buf("lf3")
            Bjunkb = Buf("junkb")
            BP = bufs("Pl", 2)
            sp.dma(bfb[:], ab_bf_in[j].partition_broadcast(128), Bbfb)
            fp32_proj8(es, ab_w_in[j][:, 1536:1544], fa, Bfa)
            for t in range(NT):
                dve.op(lambda e: e.tensor_tensor(fa[:, t, :], fa[:, t, :], bfb[:], ALU.add), R=[Bfa, Bbfb], W=[Bfa])
            faf = fa[:].rearrange("p t e -> p (t e)")
            act.op(lambda e: e.activation(faf, faf, AF.Exp, scale=-1.0), R=[Bfa], W=[Bfa])
            act.op(lambda e: e.activation(faf, faf, AF.Ln, bias=1.0), R=[Bfa], W=[Bfa])
            for g in range(4):
                for tt in range(4):
                    t = g * 4 + tt
                    pe.op(lambda e: e.transpose(P[0:8, g % 2, tt * 128:(tt + 1) * 128], fa[:, t, :], ident_f[:]),
                          R=[Bfa, Bc], W=[BP[g % 2]], signal=(tt == 3))
                act.op(lambda e: e.mul(lfT[:, g * 512:(g + 1) * 512], P[0:8, g % 2, :], -1.0), R=[BP[g % 2]], W=[BlfT])
            split3(es, "a", lfT[:], BlfT, lf3, Blf3, T)
            for jj in range(3):
                sp.dma(exm(2048 + jj * 8, 2048 + jj * 8 + 8), lf3[:, jj, :], B_exm, R=[Blf3])
            K.barrier()
        if os.environ.get("DBG_SUB") == "1":
            return
        with ExitStack() as es:
            wq = SB(es, "wq", [128, 8, 1024], BF16)
            wk = SB(es, "wk", [128, 8, 1024], BF16)
            wv = SB(es, "wv", [128, 8, 1024], BF16)
            stg = SB(es, "stg", [64, 2, T], BF16)
            vstg = SB(es, "vstg", [128, 2, 1024], BF16)
            P = PS(es, "P", [128, 2, 512], F32)
            Vp = PS(es, "Vp", [128, 2, 1024], F32)
            Bwq, Bwk, Bwv, Bwfb, Bbfb = Buf("wq"), Buf("wk"), Buf("wv"), Buf("wfb"), Buf("bfb")
            Bstg, Bvstg, BP, BVp = bufs("stg", 2), bufs("vstg", 2), bufs("P", 2), bufs("Vp", 2)
            Bjunk, Bfa, BlfT, Blf3 = Buf("junk"), Buf("fa"), Buf("lfT"), Buf("lf3")
            w = ab_w_in[j]
            for (dst, Bd, c0, c1) in ((wq, Bwq, 0, 1544), (wk, Bwk, 512, 2056), (wv, Bwv, 1024, 2568)):
                pool.dma(dst[:, :, 0:512], w[:, c0:c0 + 512].rearrange("(k p) c -> p k c", p=128), Bd)
                pool.dma(dst[:, :, 512:1024], w[:, c1:c1 + 512].rearrange("(k p) c -> p k c", p=128), Bd)
            u = 0
            for which, wt, Bw in (("q", wq, Bwq), ("k", wk, Bwk)):
                for h in range(16):
                    s = u % 2
                    for tg in range(4):
                        ps_ = (u * 4 + tg) % 2
                        for k in range(8):
                            pe.op(lambda e: e.matmul(P[0:64, ps_, :], wt[:, k, h * 64:(h + 1) * 64],
                                                     XT[:, k, tg * 512:(tg + 1) * 512], start=(k == 0), stop=(k == 7)),
                                  R=[Bw] + BXT[tg * 4:(tg + 1) * 4], W=[BP[ps_]], signal=(k == 7))
                        sc = 0.125 if which == "q" else 1.0
                        act.op(lambda e: e.mul(stg[:, s, tg * 512:(tg + 1) * 512], P[0:64, ps_, :], sc),
                               R=[BP[ps_]], W=[Bstg[s]])
                    if which == "q":
                        sp.dma(q_scr.ap()[h], stg[:, s, :], B_qscr, R=[Bstg[s]])
                    else:
                        sp.dma(exm(h * 64, (h + 1) * 64), stg[:, s, :], B_exm, R=[Bstg[s]])
                    u += 1
            for t in range(NT):
                s = t % 2
                for c in range(2):
                    for k in range(8):
                        pe.op(lambda e: e.matmul(Vp[:, s, c * 512:(c + 1) * 512], XT[:, k, t * 128:(t + 1) * 128],
                                                 wv[:, k, c * 512:(c + 1) * 512], start=(k == 0), stop=(k == 7)),
                              R=[Bwv, BXT[t]], W=[BVp[s]], signal=(k == 7 and c == 1))
                act.op(lambda e: e.copy(vstg[:, s, :], Vp[:, s, :]), R=[BVp[s]], W=[Bvstg[s]])
                sp.dma(tokview(exm(1024 + t * 64, 1024 + (t + 1) * 64)), vstg[:, s, :], B_exm, R=[Bvstg[s]])
            if os.environ.get("DBG_SUB") == "2":
                K.barrier()
                return
            allgather(ex_mine_c, ex_all_c, B_exm, B_exa)
            K.barrier()
        if os.environ.get("DBG_SUB") == "3":
            return
        with ExitStack() as es:
            l3 = SB(es, "l3", [8, 3, S], BF16)
            lfa = SB(es, "lfa", [8, S], F32)
            onesr = SB(es, "onesr", [8, S], BF16)
            cT = SB(es, "cT", [8, S], F32)
            cown = SB(es, "cown", [8, T], F32)
            cq3 = SB(es, "cq3", [8, 3, T], BF16)
            osl = SB(es, "osl", [8, 8], F32)
            Bl3, Blfa, Bones, BcT, Bcown, Bcq3, Bosl = (Buf("l3"), Buf("lfa"), Buf("onesr"), Buf("cT"),
                                                         Buf("cown"), Buf("cq3"), Buf("osl"))
            sp.dma(osl[:], osel_in[0].partition_broadcast(8), Bosl)
            for gs in range(8):
                r, li = OWNER[gs], LIDX[gs]
                for jj in range(3):
                    sp.dma(l3[:, jj, gs * 512:(gs + 1) * 512], exa(r, 2048 + jj * 8, 2048 + jj * 8 + 8)[:, li * 512:(li + 1) * 512],
                           Bl3, R=[B_exa])
            dve.op(lambda e: e.memset(onesr[:], 1.0), W=[Bones])
            dve.op(lambda e: e.tensor_tensor(lfa[:], l3[:, 0, :], l3[:, 1, :], ALU.add), R=[Bl3], W=[Blfa])
            dve.op(lambda e: e.tensor_tensor(lfa[:], lfa[:], l3[:, 2, :], ALU.add), R=[Bl3, Blfa], W=[Blfa])
            dve.op(lambda e: e.tensor_tensor_scan(cT[:], onesr[:], lfa[:], 0.0, ALU.mult, ALU.add), R=[Bones, Blfa], W=[BcT])
            split3(es, "b", cT[:], BcT, l3, Bl3, S)
            sp.dma(c_scr.ap()[:, 0:3, :], l3[:], B_cscr, R=[Bl3])
            for jj in range(3):
                dve.op(lambda e: e.tensor_scalar_mul(l3[:, jj, :], l3[:, jj, :], -1.0), R=[Bl3], W=[Bl3])
            sp.dma(c_scr.ap()[:, 3:6, :], l3[:], B_cscr, R=[Bl3])
            for ls in range(4):
                a0 = GSMIN[ls] * 512
                dve.op(lambda e: e.tensor_scalar_mul(cown[:, ls * 512:(ls + 1) * 512], cT[:, a0:a0 + 512],
                                                     osl[:, 2 * ls:2 * ls + 1]), R=[BcT, Bosl], W=[Bcown])
                dve.op(lambda e: e.scalar_tensor_tensor(cown[:, ls * 512:(ls + 1) * 512], cT[:, a0 + 512:a0 + 1024],
                                                        osl[:, 2 * ls + 1:2 * ls + 2], cown[:, ls * 512:(ls + 1) * 512],
                                                        ALU.mult, ALU.add), R=[BcT, Bosl, Bcown], W=[Bcown])
            split3(es, "c", cown[:], Bcown, cq3, Bcq3, T)
            sp.dma(cq_scr.ap(), cq3[:], B_cqscr, R=[Bcq3])
            K.barrier()

    def attn_alloc(es, nkcols, nvblk):
        d = {}
        d["qt"] = SB(es, "qt", [128, 2, T], BF16)
        d["kt"] = SB(es, "kt", [128, 2, nkcols], BF16)
        d["vt"] = SB(es, "vt", [128, 2, nvblk, 65], BF16)
        sbt = {"at": SB(es, "at", [128, 4, 512], BF16), "el": SB(es, "el", [128, 2, 512], F32),
               "ll": SB(es, "ll", [128, 2, 512], BF16), "rs": SB(es, "rs", [128, 512], F32),
               "ot": SB(es, "ot", [64, 2, T], BF16), "rd": SB(es, "rd", [128, 512], F32),
               "bc": SB(es, "bc", [64, 512], F32)}
        Bsb = {"at": bufs("at", 4), "el": bufs("el", 2), "ll": bufs("ll", 2), "rs": Buf("rs"),
               "ot": bufs("ot", 2), "rd": Buf("rd"), "bc": Buf("bc")}
        ps = {"S": PS(es, "S", [128, 2, 512], F32), "T": PS(es, "T", [128, 2, 512], F32),
              "R": PS(es, "R", [128, 512], F32), "O": PS(es, "O", [128, 2, 512], F32), "B": PS(es, "B", [128, 512], F32)}
        Bps = {"S": bufs("S", 2), "T": bufs("T", 2), "R": Buf("R"), "O": bufs("O", 2), "B": Buf("B")}
        d["Bqt"], d["Bkt"], d["Bvt"] = bufs("qt", 2), bufs("kt", 2), bufs("vt", 2)
        for s in range(2):
            dve.op(lambda e: e.memset(d["qt"][:, s, :], 0.0), W=[d["Bqt"][s]])
            pool.op(lambda e: e.memset(d["kt"][:, s, :], 0.0), W=[d["Bkt"][s]])
            dve.op(lambda e: e.memset(d["vt"][:, s, :, :], 1.0), W=[d["Bvt"][s]])
        return d, sbt, Bsb, ps, Bps

    def even_attn(j):
        with ExitStack() as es:
            d, sbt, Bsb, ps, Bps = attn_alloc(es, S, 32)
            qt, kt, vt = d["qt"], d["kt"], d["vt"]
            mle = SB(es, "mle", [128, 2, 8, 512], BF16)
            mlt = SB(es, "mlt", [128, 2, 8, 512], BF16)
            Bmle, Bmlt = Buf("mle"), Buf("mlt")
            for o in range(2):
                pool.dma(mle[:, o], mle_in[o].rearrange("r p q -> p r q"), Bmle)
                pool.dma(mlt[:, o], mlt_in[o].rearrange("r p q -> p r q"), Bmlt)
            for s in range(2):
                dve.op(lambda e: e.memset(qt[64:70, s, :], 1.0), W=[d["Bqt"][s]])
                dve.op(lambda e: e.memset(kt[64:70, s, :], 1.0), W=[d["Bkt"][s]])
            for h in range(16):
                s = h % 2
                fox = h < 8
                Bq, Bk, Bv = d["Bqt"][s], d["Bkt"][s], d["Bvt"][s]
                if h == 8 or h == 9:
                    dve.op(lambda e: e.memset(qt[64:70, s, :], 0.0), W=[Bq])
                sp.dma(qt[0:64, s, :], q_scr.ap()[h], Bq, R=[B_qscr])
                if fox:
                    sp.dma(qt[64:67, s, :], cq_scr.ap()[h], Bq, R=[B_cqscr])
                    sp.dma(kt[67:70, s, :], c_scr.ap()[h, 3:6, :], Bk, R=[B_cscr])
                for gs in range(8):
                    r, li = OWNER[gs], LIDX[gs]
                    sp.dma(kt[0:64, s, gs * 512:(gs + 1) * 512],
                           exa(r, h * 64, (h + 1) * 64)[:, li * 512:(li + 1) * 512], Bk, R=[B_exa])
                    vsec = tokview(exa(r, 1024 + li * 256, 1024 + (li + 1) * 256))
                    sp.dma(vt[:, s, gs * 4:(gs + 1) * 4, 0:64],
                           vsec[:, h * 64:(h + 1) * 64].rearrange("(j p) d -> p j d", p=128),
                           Bv, R=[B_exa])
                msk, Bm = (mle, Bmle) if fox else (mlt, Bmlt)

                def blocks(ls):
                    nkb = 4 * (GSMIN[ls] + 2)
                    res = []
                    for kb in range(nkb):
                        rr = kb - 4 * GSMIN[ls]
                        m_ap = msk[:, ls % 2, rr, :] if rr >= 0 else None
                        res.append((kb * 128, kb, m_ap, Bm))
                    return res
                attention_core("sm" if fox else "sb", qt[:, s, :], Bq, kt[:, s, :], Bk, vt[:, s], Bv, blocks,
                               ps, Bps, {**sbt, "ot": sbt["ot"][:, s, :]}, {**Bsb, "ot": Bsb["ot"][s]})
                sp.dma(ot_scr.ap()[h], sbt["ot"][:, s, :], B_otscr, R=[Bsb["ot"][s]])
            K.barrier()

    def out_proj_ln(w_out_ap, ln_idx):
        with ExitStack() as es:
            wo = SB(es, "wo", [64, 16, D], BF16)
            otl = SB(es, "otl", [64, 2, 16, 512], BF16)
            gb = SB(es, "gb", [128, 2, D], F32)
            tmp = SB(es, "tmp", [128, 2, D], F32)
            st = SB(es, "st", [128, 2, 32], F32)
            xb = SB(es, "xb", [128, 2, D], BF16)
            y_ps = PS(es, "yps", [128, 2, 1024], F32)
            tr_ps = PS(es, "trps", [128, 2, 1024], BF16)
            Bwo, Bgb = Buf("wo"), Buf("gb")
            Botl = bufs("otl", 2)
            Btmp, Bst, Bxb, By, Btr = bufs("tmp", 2), bufs("st", 2), bufs("xb", 2), bufs("y", 2), bufs("tr", 2)
            pool.dma(wo[:], w_out_ap.rearrange("(h d) n -> d h n", d=64), Bwo)

            def get_y(t, s):
                tg, tt = t // 4, t % 4
                so = tg % 2
                if tt == 0:
                    sp.dma(otl[:, so], ot_scr.ap()[:, :, tg * 512:(tg + 1) * 512].rearrange("h d t -> d h t"), Botl[so], R=[B_otscr])
                for c in range(2):
                    for h in range(16):
                        pe.op(lambda e: e.matmul(y_ps[:, s, c * 512:(c + 1) * 512], otl[:, so, h, tt * 128:(tt + 1) * 128],
                                                 wo[:, h, c * 512:(c + 1) * 512], start=(h == 0), stop=(h == 15)),
                              R=[Botl[so], Bwo], W=[By[s]], signal=(h == 15 and c == 1))
                dve.op(lambda e: e.scalar_tensor_tensor(tmp[:, s, :], X[:, t, :], ALPHA, y_ps[:, s, :], ALU.mult, ALU.add),
                       R=[BX[t], By[s]], W=[Btmp[s]])
            layer_norm_tiles(ln_idx, get_y, gb, Bgb, tmp, Btmp, st, Bst)
            make_xt(tr_ps, Btr, xb, Bxb)
            K.barrier()

    def ffn_phase(experts, d_ff, comb=None, Bcomb=None):
        with ExitStack() as es:
            wgu = SB(es, "wgu", [128, 2, 2, 8, 512], BF16)
            wdn = SB(es, "wdn", [128, 2, 4, D], BF16)
            actT = SB(es, "actT", [128, 2, 4, T], BF16)
            sg = SB(es, "sg", [128, 2, 512], F32)
            GU = PS(es, "GU", [128, 4, 512], F32)
            Y = PS(es, "Y", [128, 2, 1024], F32)
            Bwgu, Bwdn, BactT, Bsg, BGU, BY = bufs("wgu", 2), bufs("wdn", 2), bufs("actT", 2), bufs("sg", 2), bufs("GU", 2), bufs("Y", 2)
            for t in range(NT):
                dve.op(lambda e: e.tensor_scalar_mul(X[:, t, :], X[:, t, :], ALPHA), R=[BX[t]], W=[BX[t]])
            nch = d_ff // 128
            groups = [(g0, min(4, nch - g0)) for g0 in range(0, nch, 4)]
            gi = 0
            cnt = 0
            for ei, (wg_ap, wd_ap) in enumerate(experts):
                for (g0, gn) in groups:
                    s = gi % 2
                    c0 = g0 * 128
                    pool.dma(wgu[:, s, 0, :, 0:gn * 128],
                             wg_ap[:, c0:c0 + gn * 128].rearrange("(k p) n -> p k n", p=128), Bwgu[s])
                    pool.dma(wgu[:, s, 1, :, 0:gn * 128],
                             wg_ap[:, d_ff + c0:d_ff + c0 + gn * 128].rearrange("(k p) n -> p k n", p=128), Bwgu[s])
                    pool.dma(wdn[:, s, 0:gn, :], wd_ap[c0:c0 + gn * 128, :].rearrange("(c p) n -> p c n", p=128), Bwdn[s])
                    for c in range(gn):
                        for tg in range(4):
                            ps_ = cnt % 2
                            cnt += 1
                            for half in range(2):
                                for k in range(8):
                                    pe.op(lambda e: e.matmul(GU[:, ps_ * 2 + half, :], wgu[:, s, half, k, c * 128:(c + 1) * 128],
                                                             XT[:, k, tg * 512:(tg + 1) * 512], start=(k == 0), stop=(k == 7)),
                                          R=[Bwgu[s]] + BXT[tg * 4:(tg + 1) * 4], W=[BGU[ps_]], signal=(k == 7 and half == 1))
                            act.op(lambda e: e.activation(sg[:, ps_, :], GU[:, ps_ * 2, :], AF.Silu), R=[BGU[ps_]], W=[Bsg[ps_]])
                            dve.op(lambda e: e.tensor_tensor(actT[:, s, c, tg * 512:(tg + 1) * 512], sg[:, ps_, :],
                                                             GU[:, ps_ * 2 + 1, :], ALU.mult),
                                   R=[Bsg[ps_], BGU[ps_]], W=[BactT[s]])
                    for t in range(NT):
                        ys = t % 2
                        for cc in range(2):
                            for c in range(gn):
                                pe.op(lambda e: e.matmul(Y[:, ys, cc * 512:(cc + 1) * 512], actT[:, s, c, t * 128:(t + 1) * 128],
                                                         wdn[:, s, c, cc * 512:(cc + 1) * 512], start=(c == 0), stop=(c == gn - 1)),
                                      R=[BactT[s], Bwdn[s]], W=[BY[ys]], signal=(c == gn - 1 and cc == 1))
                        if comb is None:
                            dve.op(lambda e: e.tensor_tensor(X[:, t, :], X[:, t, :], Y[:, ys, :], ALU.add),
                                   R=[BX[t], BY[ys]], W=[BX[t]])
                        else:
                            dve.op(lambda e: e.scalar_tensor_tensor(X[:, t, :], Y[:, ys, :], comb[:, t, ei:ei + 1], X[:, t, :],
                                                                    ALU.mult, ALU.add),
                                   R=[BX[t], BY[ys], Bcomb], W=[BX[t]])
                    gi += 1
            K.barrier()

    def ln2_ple(i):
        with ExitStack() as es:
            gb = SB(es, "gb2", [128, 2, D], F32)
            tmp = SB(es, "tmp2", [128, 2, D], F32)
            st = SB(es, "st2", [128, 2, 32], F32)
            xb = SB(es, "xb2", [128, 2, D], BF16)
            wg = SB(es, "wg2", [128, 8, D], BF16)
            wp = SB(es, "wp2", [128, 2, D], BF16)
            pb = SB(es, "pb2", [128, 2, 256], BF16)
            pT = SB(es, "pT2", [128, 2, 2, 128], BF16)
            sig = SB(es, "sig2", [128, 2, D], F32)
            tr_ps = PS(es, "trps2", [128, 2, 1024], BF16)
            G = PS(es, "G2", [128, 1024], F32)
            PP = PS(es, "PP2", [128, 1024], F32)
            ptr = PS(es, "ptr2", [128, 1024], BF16)
            Bgb, Bwg, Bwp = Buf("gb"), Buf("wg"), Buf("wp")
            Btmp, Bst, Bxb, Btr, Bpb, BpT, Bsig = (bufs("tmp", 2), bufs("st", 2), bufs("xb", 2), bufs("tr", 2), bufs("pb", 2),
                                                   bufs("pT", 2), bufs("sig", 2))
            BG, BPP, Bptr = Buf("G"), Buf("PP"), Buf("ptr")
            pool.dma(wg[:], ple_g[i].rearrange("(k p) n -> p k n", p=128), Bwg)
            pool.dma(wp[:], ple_p[i].rearrange("(k p) n -> p k n", p=128), Bwp)

            def get_y(t, s):
                dve.op(lambda e: e.tensor_copy(tmp[:, s, :], X[:, t, :]), R=[BX[t]], W=[Btmp[s]])
            layer_norm_tiles(2 * i + 1, get_y, gb, Bgb, tmp, Btmp, st, Bst)
            make_xt(tr_ps, Btr, xb, Bxb)
            for t in range(NT):
                s = t % 2
                pool.dma(pb[:, s, :], p_in[i, t * 128:(t + 1) * 128, :], Bpb[s])
                for k in range(2):
                    pe.op(lambda e: e.transpose(ptr[:, k * 128:(k + 1) * 128], pb[:, s, k * 128:(k + 1) * 128], ident_b[:]),
                          R=[Bpb[s], Bc], W=[Bptr], signal=(k == 1))
                act.op(lambda e: e.copy(pT[:, s, :, :], ptr[:, 0:256].rearrange("p (k n) -> p k n", k=2)), R=[Bptr], W=[BpT[s]])
                for c in range(2):
                    for k in range(8):
                        pe.op(lambda e: e.matmul(G[:, c * 512:(c + 1) * 512], XT[:, k, t * 128:(t + 1) * 128],
                                                 wg[:, k, c * 512:(c + 1) * 512], start=(k == 0), stop=(k == 7)),
                              R=[BXT[t], Bwg], W=[BG], signal=(k == 7 and c == 1))
                for c in range(2):
                    for k in range(2):
                        pe.op(lambda e: e.matmul(PP[:, c * 512:(c + 1) * 512], pT[:, s, k, :],
                                                 wp[:, k, c * 512:(c + 1) * 512], start=(k == 0), stop=(k == 1)),
                              R=[BpT[s], Bwp], W=[BPP], signal=(k == 1 and c == 1))
                act.op(lambda e: e.activation(sig[:, s, :], G[:, :], AF.Sigmoid), R=[BG], W=[Bsig[s]])
                dve.op(lambda e: e.tensor_tensor(sig[:, s, :], sig[:, s, :], PP[:, :], ALU.mult), R=[Bsig[s], BPP], W=[Bsig[s]])
                dve.op(lambda e: e.tensor_tensor(X[:, t, :], X[:, t, :], sig[:, s, :], ALU.add), R=[BX[t], Bsig[s]], W=[BX[t]])
            make_xt(tr_ps, Btr, xb, Bxb)
            K.barrier()

    def odd_proj_attn(j):
        with ExitStack() as es:
            wqkv = SB(es, "wqkv", [128, 8, 1280], BF16)
            cst = SB(es, "cst", [128, 2, NT, 8], F32)
            qkv = SB(es, "qkv", [128, 2, 1280], F32)
            rt = SB(es, "rt", [128, 4, 18, 8], F32)
            qb = SB(es, "qb", [128, 2, 1280], BF16)
            qstg = SB(es, "qstg", [64, 2, 8, 128], BF16)
            QKV = PS(es, "QKV", [128, 3, 512], F32)
            TR = PS(es, "TR", [128, 2, 1024], BF16)
            Bw, Bcs, Brt = Buf("wqkv"), Buf("cst"), Buf("rt")
            Bqkv, Bqb, Bqstg, BTR = bufs("qkv", 2), bufs("qb", 2), bufs("qstg", 2), bufs("TR", 2)
            BQKV = Buf("QKV")
            pool.dma(wqkv[:], c_w_qkv[j].rearrange("(k p) n -> p k n", p=128), Bw)
            sp.dma(cst[:, 0], cos_in.rearrange("(t p) e -> p t e", p=128), Bcs)
            sp.dma(cst[:, 1], sin_in.rearrange("(t p) e -> p t e", p=128), Bcs)
            vsec = ex2_mine.ap()[128:256, :].rearrange("r (a c) -> (r a) c", c=128)
            gcount = 0
            for t in range(NT):
                s = t % 2
                for c, (c0, c1) in enumerate(((0, 512), (512, 1024), (1024, 1280))):
                    for k in range(8):
                        pe.op(lambda e: e.matmul(QKV[:, c, 0:c1 - c0], XT[:, k, t * 128:(t + 1) * 128], wqkv[:, k, c0:c1],
                                                 start=(k == 0), stop=(k == 7)),
                              R=[BXT[t], Bw], W=[BQKV], signal=(k == 7 and c == 2))
                for c, (c0, c1) in enumerate(((0, 512), (512, 1024), (1024, 1280))):
                    act.op(lambda e: e.copy(qkv[:, s, c0:c1], QKV[:, c, 0:c1 - c0]), R=[BQKV], W=[Bqkv[s]])
                hv = qkv[:, s, 0:1152].rearrange("p (h d) -> p h d", d=64)
                x1, x2 = hv[:, :, 0:8], hv[:, :, 8:16]
                cosb = cst[:, 0, t:t + 1, :].to_broadcast([128, 18, 8])
                sinb = cst[:, 1, t:t + 1, :].to_broadcast([128, 18, 8])
                dve.op(lambda e: e.tensor_tensor(rt[:, 0], x1, cosb, ALU.mult), R=[Bqkv[s], Bcs], W=[Brt])
                dve.op(lambda e: e.tensor_tensor(rt[:, 1], x2, sinb, ALU.mult), R=[Bqkv[s], Bcs], W=[Brt])
                dve.op(lambda e: e.tensor_tensor(rt[:, 2], x2, cosb, ALU.mult), R=[Bqkv[s], Bcs], W=[Brt])
                dve.op(lambda e: e.tensor_tensor(rt[:, 3], x1, sinb, ALU.mult), R=[Bqkv[s], Bcs], W=[Brt])
                dve.op(lambda e: e.tensor_tensor(x1, rt[:, 0], rt[:, 1], ALU.subtract), R=[Brt], W=[Bqkv[s]])
                dve.op(lambda e: e.tensor_tensor(x2, rt[:, 2], rt[:, 3], ALU.add), R=[Brt], W=[Bqkv[s]])
                act.op(lambda e: e.mul(qb[:, s, 0:1024], qkv[:, s, 0:1024], 0.125), R=[Bqkv[s]], W=[Bqb[s]])
                act.op(lambda e: e.copy(qb[:, s, 1024:1280], qkv[:, s, 1024:1280]), R=[Bqkv[s]], W=[Bqb[s]])
                for g in range(3):
                    gs_ = gcount % 2
                    gcount += 1
                    nh = 8 if g < 2 else 2
                    for hh in range(nh):
                        h = g * 8 + hh
                        pe.op(lambda e: e.transpose(TR[0:64, gs_, hh * 128:(hh + 1) * 128], qb[:, s, h * 64:(h + 1) * 64], ident_b[:]),
                              R=[Bqb[s], Bc], W=[BTR[gs_]], signal=(hh == nh - 1))
                    dve.op(lambda e: e.tensor_copy(qstg[:, gs_, 0:nh, :],
                                                   TR[0:64, gs_, 0:nh * 128].rearrange("p (h n) -> p h n", n=128)),
                           R=[BTR[gs_]], W=[Bqstg[gs_]])
                    if g < 2:
                        sp.dma(q_scr.ap()[g * 8:(g + 1) * 8, :, t * 128:(t + 1) * 128].rearrange("h d n -> d h n"),
                               qstg[:, gs_, :, :], B_qscr, R=[Bqstg[gs_]])
                    else:
                        sp.dma(ex2_mine.ap()[0:128, t * 128:(t + 1) * 128].rearrange("(h d) n -> d h n", d=64),
                               qstg[:, gs_, 0:2, :], B_ex2m, R=[Bqstg[gs_]])
                sp.dma(vsec[t * 128:(t + 1) * 128, :], qb[:, s, 1152:1280], B_ex2m, R=[Bqb[s]])
            allgather(ex2_mine, ex2_all, B_ex2m, B_ex2a)
            K.barrier()
        with ExitStack() as es:
            d, sbt, Bsb, ps, Bps = attn_alloc(es, T + 7 * 128, NT + 7)
            qt, kt, vt = d["qt"], d["kt"], d["vt"]
            msw = SB(es, "msw", [128, 4, 6, 512], BF16)
            sk = SB(es, "sk", [128, 16], F32)
            Bmsw, Bsk = Buf("msw"), Buf("sk")
            for ls in range(4):
                pool.dma(msw[:, ls], msw_in[ls].rearrange("r p q -> p r q"), Bmsw)
            sp.dma(sk[:], c_sinks[j].partition_broadcast(128), Bsk)
            act.op(lambda e: e.activation(sk[:], sk[:], AF.Exp), R=[Bsk], W=[Bsk])
            cands = [c for l in SWA_CANDS for c in l]
            for kv in range(2):
                Bk, Bv = d["Bkt"][kv], d["Bvt"][kv]
                sp.dma(kt[0:64, kv, 0:T], ex2_mine.ap()[kv * 64:(kv + 1) * 64, :], Bk, R=[B_ex2m])
                sp.dma(vt[:, kv, 0:NT, 0:64], vsec_of(ex2_mine.ap(), 0)[:, kv * 64:(kv + 1) * 64].rearrange("(j p) d -> p j d", p=128),
                       Bv, R=[B_ex2m])
                for ci, (r, li) in enumerate(cands):
                    t0 = li * 512 + 384
                    sp.dma(kt[0:64, kv, T + ci * 128:T + (ci + 1) * 128],
                           ex2_all.ap()[r * 256 + kv * 64:r * 256 + (kv + 1) * 64, t0:t0 + 128], Bk, R=[B_ex2a])
                    sp.dma(vt[:, kv, NT + ci, 0:64], vsec_of(ex2_all.ap(), r * 256)[t0:t0 + 128, kv * 64:(kv + 1) * 64], Bv, R=[B_ex2a])
            cbase = [0, 1, 3, 5]
            for h in range(16):
                s = h % 2
                kv = h // 8
                Bq = d["Bqt"][s]
                sp.dma(qt[0:64, s, :], q_scr.ap()[h], Bq, R=[B_qscr])

                def blocks(ls):
                    res = []
                    for jj in range(4):
                        res.append(((4 * ls + jj) * 128, 4 * ls + jj, msw[:, ls, jj, :], Bmsw))
                    for cc in range(len(SWA_CANDS[ls])):
                        ci = cbase[ls] + cc
                        res.append((T + ci * 128, NT + ci, msw[:, ls, 4 + cc, :], Bmsw))
                    return res
                attention_core("sm", qt[:, s, :], Bq, kt[:, kv, :], d["Bkt"][kv], vt[:, kv], d["Bvt"][kv], blocks,
                               ps, Bps, {**sbt, "ot": sbt["ot"][:, s, :]}, {**Bsb, "ot": Bsb["ot"][s]},
                               sink_ap=sk[64:65, h:h + 1], Bsink=Bsk)
                sp.dma(ot_scr.ap()[h], sbt["ot"][:, s, :], B_otscr, R=[Bsb["ot"][s]])
            K.barrier()

    def vsec_of(ap, row0):
        return ap[row0 + 128:row0 + 256, :].rearrange("r (a c) -> (r a) c", c=128)

    def moe_phase(j):
        with ExitStack() as es:
            comb = SB(es, "comb", [128, NT, 8], F32)
            Bcomb = Buf("comb")
            with ExitStack() as es2:
                rbb = SB(es2, "rbb", [128, 8], F32)
                lg = SB(es2, "lg", [128, NT, 8], F32)
                mx = SB(es2, "mx", [128, NT, 8], F32)
                gg = SB(es2, "gg", [128, NT, 4], F32)
                mm = SB(es2, "mm", [128, NT, 8], F32)
                Brtb, Brbb, Blg, Bmx, Bgg, Bmm, Bjunk = Buf("rtb"), Buf("rbb"), Buf("lg"), Buf("mx"), Buf("gg"), Buf("mm"), bufs("junk", 2)
                Bjunkb = Buf("junkb")
                sp.dma(rbb[:], rb_in[j].partition_broadcast(128), Brbb)
                fp32_proj8(es2, router_in[j], lg, Blg)
                for t in range(NT):
                    dve.op(lambda e: e.tensor_tensor(lg[:, t, :], lg[:, t, :], rbb[:], ALU.add), R=[Blg, Brbb], W=[Blg])
                    dve.op(lambda e: e.max(mx[:, t, :], lg[:, t, :]), R=[Blg], W=[Bmx])
                    dve.op(lambda e: e.tensor_tensor(gg[:, t, 0:1], mx[:, t, 1:2], mx[:, t, 0:1], ALU.subtract), R=[Bmx], W=[Bgg])
                    act.op(lambda e: e.activation(gg[:, t, 1:2], gg[:, t, 0:1], AF.Sigmoid), R=[Bgg], W=[Bgg])
                    dve.op(lambda e: e.tensor_scalar(gg[:, t, 2:3], gg[:, t, 1:2], -1.0, 1.0, ALU.mult, ALU.add), R=[Bgg], W=[Bgg])
                    dve.op(lambda e: e.tensor_scalar(mm[:, t, :], lg[:, t, :], mx[:, t, 0:1], gg[:, t, 2:3], ALU.is_equal, ALU.mult),
                           R=[Blg, Bmx, Bgg], W=[Bmm])
                    dve.op(lambda e: e.tensor_scalar(comb[:, t, :], lg[:, t, :], mx[:, t, 1:2], gg[:, t, 1:2], ALU.is_equal, ALU.mult),
                           R=[Blg, Bmx, Bgg], W=[Bcomb])
                    dve.op(lambda e: e.tensor_tensor(comb[:, t, :], comb[:, t, :], mm[:, t, :], ALU.add), R=[Bmm, Bcomb], W=[Bcomb])
                K.barrier()
            ffn_phase([(moe_gu[j, e_], moe_dn[j, e_]) for e_ in range(8)], D_FFE, comb, Bcomb)

    def dump_x():
        for t in range(NT):
            sp.dma(out[t * 128:(t + 1) * 128, :], X[:, t, :], B_out, R=[BX[t]])
        for e in K.engs:
            e.wait(B_out.w)

    with ExitStack() as es0:
        xb0 = SB(es0, "xb0", [128, 2, D], BF16)
        tr0 = PS(es0, "tr0", [128, 2, 1024], BF16)
        make_xt(tr0, bufs("tr0", 2), xb0, bufs("xb0", 2))
        K.barrier()

    phases = []
    for i in range(layer0, layer0 + n_layers):
        j = i // 2
        if i % 2 == 0:
            phases += [lambda j=j: even_proj(j), lambda j=j: even_attn(j), lambda i=i, j=j: out_proj_ln(ab_w_out[j], 2 * i),
                       lambda j=j: ffn_phase([(ffn_gu[j], ffn_dn[j])], D_FF)]
        else:
            phases += [lambda j=j: odd_proj_attn(j), lambda i=i, j=j: out_proj_ln(c_w_out[j], 2 * i), lambda j=j: moe_phase(j)]
        phases.append(lambda i=i: ln2_ple(i))
    if dbg_stage is not None:
        phases = phases[:dbg_stage]
    for ph in phases:
        ph()
    dump_x()
    return nc


def _host_consts(parity):
    gs_own = OWN[parity]
    kk = np.arange(128)[:, None]
    qq = np.arange(512)[None, :]
    mle = np.zeros((2, 8, 128, 512), np.float32)
    mlt = np.zeros((2, 8, 128, 512), np.float32)
    for o in range(2):
        gs = gs_own[o]
        for r in range(8):
            kpos = (4 * GSMIN[o] + r) * 128 + kk
            qpos = gs * 512 + qq
            mle[o, r] = np.where(kpos <= qpos, 0.0, NEG)
            mlt[o, r] = np.where(kpos < qpos, 0.0, NEG)
    osel = np.zeros((1, 8), np.float32)
    for ls in range(4):
        o = gs_own[ls] - GSMIN[ls]
        osel[0, 2 * ls] = 1.0 - o
        osel[0, 2 * ls + 1] = float(o)
    msw = np.full((4, 6, 128, 512), NEG, np.float32)
    for ls in range(4):
        for jj in range(4):
            diff = qq - (jj * 128 + kk)
            msw[ls, jj] = np.where((diff >= 0) & (diff < 128), 0.0, NEG)
        prev = gs_own[ls] - 1
        for cc, (r, li) in enumerate(SWA_CANDS[ls]):
            real = prev >= 0 and OWNER[prev] == r and LIDX[prev] == li
            if real:
                diff = qq - (kk - 128)
                msw[ls, 4 + cc] = np.where((diff >= 0) & (diff < 128), 0.0, NEG)
    pos = np.concatenate([np.arange(g * 512, (g + 1) * 512) for g in gs_own]).astype(np.float32)
    inv = (500000.0 ** (-np.arange(8, dtype=np.float32) * 2.0 / 16.0)).astype(np.float32)
    ang = pos[:, None] * inv[None, :]
    cos = np.cos(ang).astype(np.float32)
    sin = np.sin(ang).astype(np.float32)
    return dict(mask_le=mle, mask_lt=mlt, osel=osel, mask_swa=msw, rope_cos=cos, rope_sin=sin)


_CACHE = {}


N_SPLIT = 1


def _run(inputs, n_layers=DEPTH, dbg_stage=None, trace=False, n_split=None):
    f = lambda a: np.ascontiguousarray(np.asarray(a, dtype=np.float32))
    x = f(inputs["x"])
    p = f(inputs["p"])
    if n_split is None:
        n_split = N_SPLIT if (n_layers == DEPTH and dbg_stage is None) else 1
    lng = np.empty((2 * DEPTH, D), np.float32)
    lnb = np.empty((2 * DEPTH, D), np.float32)
    lng[0::2] = f(inputs["ln_mix_g"]); lng[1::2] = f(inputs["ln_ffn_g"])
    lnb[0::2] = f(inputs["ln_mix_b"]); lnb[1::2] = f(inputs["ln_ffn_b"])
    ab_w_in = f(inputs["ab_w_in"])
    shared = dict(
        ln_g=lng, ln_b=lnb, ab_w_in=ab_w_in,
        ab_wfT=np.ascontiguousarray(ab_w_in[:, :, 1536:1544].transpose(0, 2, 1)),
        ab_b_f=f(inputs["ab_b_f"]), ab_w_out=f(inputs["ab_w_out"]), c_w_qkv=f(inputs["c_w_qkv"]),
        c_sinks=f(inputs["c_sinks"]), c_w_out=f(inputs["c_w_out"]), ffn_w_gate_up=f(inputs["ffn_w_gate_up"]),
        ffn_w_down=f(inputs["ffn_w_down"]),
        routerT=np.ascontiguousarray(f(inputs["router_w"]).transpose(0, 2, 1)), router_w=f(inputs["router_w"]),
        router_b=f(inputs["router_b"]),
        moe_w_gate_up=f(inputs["moe_w_gate_up"]), moe_w_down=f(inputs["moe_w_down"]),
        ple_w_gate=f(inputs["ple_w_gate"]), ple_w_proj=f(inputs["ple_w_proj"]),
        ident=np.eye(128, dtype=np.float32),
        triu_neg=np.where(np.arange(128)[:, None] >= np.arange(128)[None, :], -1.0, 0.0).astype(np.float32),
    )
    consts = [_host_consts(0), _host_consts(1)]
    idxs = [np.concatenate([np.arange(g * 512, (g + 1) * 512) for g in OWN[par]]) for par in range(2)]
    x_own = [np.ascontiguousarray(x[c // 2, idxs[c % 2]]) for c in range(8)]
    per = n_layers // n_split
    res = None
    for li in range(n_split):
        key = (per, dbg_stage, li * per)
        if key not in _CACHE:
            _CACHE[key] = build(per, dbg_stage, li * per)
        nc = _CACHE[key]
        in_maps = []
        for c in range(8):
            b, par = c // 2, c % 2
            m = dict(shared)
            m["x_own"] = x_own[c]
            m["p_own"] = np.ascontiguousarray(p[:, b, idxs[par]])
            m.update(consts[par])
            in_maps.append(m)
        res = run_bass_kernel_spmd(nc, in_maps, core_ids=list(range(8)), trace=trace)
        x_own = [np.ascontiguousarray(np.asarray(res.results[c]["out"], dtype=np.float32)) for c in range(8)]
    outp = np.empty((4, S, D), np.float32)
    for c in range(8):
        outp[c // 2, idxs[c % 2]] = x_own[c]
    return outp, res


def kernel(**inputs):
    outp, _ = _run(inputs)
    return outp
```

```python
import os
import numpy as np
import concourse.bass as bass
import concourse.mybir as mybir
from concourse.bass_utils import run_bass_kernel_spmd

F32 = mybir.dt.float32
BF16 = mybir.dt.bfloat16
AF = mybir.ActivationFunctionType
ALU = mybir.AluOpType
AX = mybir.AxisListType

D = 1024
S = 4096
DEPTH = 4
T = 2048
NT = 16
ALPHA = (2.0 * DEPTH) ** 0.25
EPS = 1e-5
OWN = [[0, 3, 4, 7], [1, 2, 5, 6]]
OWNER = [0, 1, 1, 0, 0, 1, 1, 0]
LIDX = [0, 0, 1, 1, 2, 2, 3, 3]
GSMIN = [0, 2, 4, 6]
NEG = -30000.0
D_FF = 2816
D_FFE = 3584
EXR = 2072
SWA_CANDS = [[(0, 0)], [(1, 1), (1, 0)], [(0, 1), (0, 2)], [(1, 3), (1, 2)]]


class Buf:
    __slots__ = ("name", "w", "r", "dsem", "dcnt", "persist", "kind")

    def __init__(self, name, persist=False):
        self.name = name
        self.w = None
        self.r = {}
        self.dsem = None
        self.dcnt = 0
        self.persist = persist
        self.kind = None


class Eng:
    def __init__(self, K, name, e, is_pe=False):
        self.K = K
        self.name = name
        self.e = e
        self.sem = K.nc.alloc_semaphore("es_" + name)
        self.count = 0
        self.waited = {}
        self.is_pe = is_pe

    def wait(self, ev):
        sem, val = ev
        if self.is_pe and sem is self.sem:
            return
        owner = self.K.sem_owner.get(sem)
        if owner is not None:
            val = max(val, 16 * owner.dcnt)
        if self.waited.get(sem, 0) >= val:
            return
        self.waited[sem] = val
        self.e.wait_ge(sem, val)

    def op(self, fn, R=(), W=(), signal=True):
        for b in R:
            if b.w is not None:
                self.wait(b.w)
        for b in W:
            if b.w is not None:
                self.wait(b.w)
            for ev in b.r.values():
                self.wait(ev)
        ins = fn(self.e)
        if signal:
            self.count += 1
            ins.then_inc(self.sem, 1)
            ev = (self.sem, self.count)
        else:
            ev = (self.sem, self.count + 1)
        for b in R:
            b.r[self.name] = ev
        for b in W:
            b.w = ev
            b.r = {}
        return ins

    def dma(self, out, in_, W, R=(), **kw):
        K = self.K
        kind = "sw" if self is K.pool else "hw"
        if W.kind is None:
            W.kind = kind
        assert W.kind == kind, (W.name, W.kind, kind)
        if W.dsem is None:
            if K.sem_pool[kind] and not W.persist:
                W.dsem, W.dcnt = K.sem_pool[kind].pop()
            else:
                W.dsem = K.nc.alloc_semaphore("ds_%d" % K.nsem)
                K.nsem += 1
            K.dbufs.append(W)
            K.sem_owner[W.dsem] = W
        for b in R:
            if b.w is not None:
                self.wait(b.w)
        same = (W.w is not None and W.w[0] is W.dsem and not W.r)
        if not same:
            if W.w is not None:
                self.wait(W.w)
            for ev in W.r.values():
                self.wait(ev)
        W.dcnt += 1
        self.e.dma_start(out=out, in_=in_, **kw).then_inc(W.dsem, 16)
        ev = (W.dsem, 16 * W.dcnt)
        for b in R:
            b.r[("d", W.dsem)] = ev
        W.w = ev
        W.r = {}


class Kern:
    def __init__(self, nc):
        self.nc = nc
        self.dbufs = []
        self.sem_pool = {"sw": [], "hw": []}
        self.sem_owner = {}
        self.nsem = 0
        self.pe = Eng(self, "pe", nc.tensor, is_pe=True)
        self.act = Eng(self, "act", nc.scalar)
        self.dve = Eng(self, "dve", nc.vector)
        self.pool = Eng(self, "pool", nc.gpsimd)
        self.sp = Eng(self, "sp", nc.sync)
        self.engs = [self.pe, self.act, self.dve, self.pool, self.sp]
        self.extra_evs = []

    def barrier(self):
        evs = [(e.sem, e.count) for e in self.engs if e.count > 0]
        evs += [(b.dsem, 16 * b.dcnt) for b in self.dbufs if b.dcnt > 0]
        evs += self.extra_evs
        for e in self.engs:
            for ev in evs:
                if ev[0] is e.sem:
                    continue
                e.wait(ev)
        keep = []
        for b in self.dbufs:
            if b.persist:
                keep.append(b)
            else:
                self.sem_pool[b.kind].append((b.dsem, b.dcnt))
                b.dsem = None
        self.dbufs = keep


def bufs(prefix, n, persist=False):
    return [Buf("%s%d" % (prefix, i), persist) for i in range(n)]


def build(n_layers=DEPTH, dbg_stage=None, layer0=0):
    from contextlib import ExitStack
    nc = bass.Bass("TRN2", target_bir_lowering=False)
    K = Kern(nc)
    pe, act, dve, pool, sp = K.pe, K.act, K.dve, K.pool, K.sp

    def din(name, shape, dt=F32):
        return nc.dram_tensor(name, list(shape), dt, kind="ExternalInput").ap()

    x_in = din("x_own", [T, D])
    p_in = din("p_own", [DEPTH, T, 256])
    lng_in = din("ln_g", [2 * DEPTH, D])
    lnb_in = din("ln_b", [2 * DEPTH, D])
    ab_w_in = din("ab_w_in", [2, D, 3080])
    wfT_in = din("ab_wfT", [2, 8, D])
    ab_bf_in = din("ab_b_f", [2, 8])
    ab_w_out = din("ab_w_out", [2, D, D])
    c_w_qkv = din("c_w_qkv", [2, D, 1280])
    c_sinks = din("c_sinks", [2, 16])
    c_w_out = din("c_w_out", [2, D, D])
    ffn_gu = din("ffn_w_gate_up", [2, D, 2 * D_FF])
    ffn_dn = din("ffn_w_down", [2, D_FF, D])
    rT_in = din("routerT", [2, 8, D])
    router_in = din("router_w", [2, D, 8])
    rb_in = din("router_b", [2, 8])
    moe_gu = din("moe_w_gate_up", [2, 8, D, 2 * D_FFE])
    moe_dn = din("moe_w_down", [2, 8, D_FFE, D])
    ple_g = din("ple_w_gate", [DEPTH, D, D])
    ple_p = din("ple_w_proj", [DEPTH, 256, D])
    mle_in = din("mask_le", [2, 8, 128, 512])
    mlt_in = din("mask_lt", [2, 8, 128, 512])
    osel_in = din("osel", [1, 8])
    msw_in = din("mask_swa", [4, 6, 128, 512])
    cos_in = din("rope_cos", [T, 8])
    sin_in = din("rope_sin", [T, 8])
    id_in = din("ident", [128, 128])
    triu_in = din("triu_neg", [128, 128])
    out = nc.dram_tensor("out", [T, D], F32, kind="ExternalOutput").ap()

    EXC = [512, 512, 512, 512, 24]
    ex_mine_c = [nc.dram_tensor("ex_mine%d" % i, [n, T], BF16) for i, n in enumerate(EXC)]
    ex_all_c = [nc.dram_tensor("ex_all%d" % i, [2 * n, T], BF16) for i, n in enumerate(EXC)]

    def exm(r0, r1):
        i = r0 // 512
        assert (r1 - 1) // 512 == i
        return ex_mine_c[i].ap()[r0 - i * 512:r1 - i * 512, :]

    def exa(rank, r0, r1):
        i = r0 // 512
        assert (r1 - 1) // 512 == i
        b = rank * EXC[i] - i * 512
        return ex_all_c[i].ap()[b + r0:b + r1, :]

    def tokview(ap2d):
        return ap2d.rearrange("r (a c) -> (r a) c", a=2)
    ex2_mine = nc.dram_tensor("ex2_mine", [256, T], BF16)
    ex2_all = nc.dram_tensor("ex2_all", [512, T], BF16)
    q_scr = nc.dram_tensor("q_scr", [16, 64, T], BF16)
    c_scr = nc.dram_tensor("c_scr", [8, 6, S], BF16)
    cq_scr = nc.dram_tensor("cq_scr", [8, 3, T], BF16)
    ot_scr = nc.dram_tensor("ot_scr", [16, 64, T], BF16)
    B_exm, B_exa, B_ex2m, B_ex2a = Buf("exm", True), Buf("exa", True), Buf("ex2m", True), Buf("ex2a", True)
    B_qscr, B_cscr, B_otscr, B_out, B_cqscr = Buf("qscr", True), Buf("cscr", True), Buf("otscr", True), Buf("out", True), Buf("cqscr", True)
    cc_sem = nc.alloc_semaphore("cc_sem")
    cc_cnt = [0]

    def allgather(src, dst, Bsrc, Bdst):
        if Bsrc.w is not None:
            pool.wait(Bsrc.w)
        if Bdst.w is not None:
            pool.wait(Bdst.w)
        for ev in Bdst.r.values():
            pool.wait(ev)
        srcs = src if isinstance(src, list) else [src]
        dsts = dst if isinstance(dst, list) else [dst]
        for s_, d_ in zip(srcs, dsts):
            cc_cnt[0] += 1
            pool.e.collective_compute(
                "AllGather", ALU.bypass,
                replica_groups=[[0, 1], [2, 3], [4, 5], [6, 7]],
                ins=[s_.ap().opt()], outs=[d_.ap().opt()],
            ).then_inc(cc_sem, 1)
        ev = (cc_sem, cc_cnt[0])
        K.extra_evs.append(ev)
        Bsrc.r["cc"] = ev
        Bdst.w = ev
        Bdst.r = {}

    X = nc.alloc_sbuf_tensor("X", [128, NT, D], F32)
    XT = nc.alloc_sbuf_tensor("XT", [128, 8, T], BF16)
    ident_b = nc.alloc_sbuf_tensor("ident_b", [128, 128], BF16)
    ident_f = nc.alloc_sbuf_tensor("ident_f", [128, 128], F32)
    triu = nc.alloc_sbuf_tensor("triu", [128, 128], BF16)
    negones = nc.alloc_sbuf_tensor("negones", [128, 128], BF16)
    ones_f = nc.alloc_sbuf_tensor("ones_f", [128, 64], F32)
    BX = bufs("X", NT, True)
    BXT = bufs("XT", NT, True)
    Bc = Buf("consts", True)
    BXld = Buf("xld", True)

    pool.dma(ident_b[:], id_in[:, :], Bc)
    pool.dma(ident_f[:], id_in[:, :], Bc)
    pool.dma(triu[:], triu_in[:, :], Bc)
    dve.op(lambda e: e.memset(negones[:], -1.0), W=[Bc])
    dve.op(lambda e: e.memset(ones_f[:], 1.0), W=[Bc])
    for t in range(NT):
        sp.dma(X[:, t, :], x_in[t * 128:(t + 1) * 128, :], BXld)
    for t in range(NT):
        BX[t].w = BXld.w

    def make_xt(ps_tr, Bps, xb, Bxb):
        for t in range(NT):
            s = t % 2
            act.op(lambda e: e.copy(xb[:, s, :], X[:, t, :]), R=[BX[t]], W=[Bxb[s]])
            for k in range(8):
                pe.op(lambda e: e.transpose(ps_tr[:, s, k * 128:(k + 1) * 128], xb[:, s, k * 128:(k + 1) * 128], ident_b[:]),
                      R=[Bxb[s], Bc], W=[Bps[s]], signal=(k == 7))
            dve.op(lambda e: e.tensor_copy(XT[:, :, t * 128:(t + 1) * 128],
                                           ps_tr[:, s, :].rearrange("p (k n) -> p k n", k=8)),
                   R=[Bps[s]], W=[BXT[t]])

    def layer_norm_tiles(idx, get_y, gb, Bgb, tmp, Btmp, st, Bst):
        sp.dma(gb[:, 0, :], lng_in[idx:idx + 1, :].partition_broadcast(128), Bgb)
        sp.dma(gb[:, 1, :], lnb_in[idx:idx + 1, :].partition_broadcast(128), Bgb)
        for t in range(NT):
            s = t % 2
            get_y(t, s)
            for c in range(2):
                dve.op(lambda e: e.bn_stats(st[:, s, c * 6:(c + 1) * 6], tmp[:, s, c * 512:(c + 1) * 512]),
                       R=[Btmp[s]], W=[Bst[s]])
            dve.op(lambda e: e.bn_aggr(st[:, s, 12:14], st[:, s, 0:12]), R=[Bst[s]], W=[Bst[s]])
            dve.op(lambda e: e.tensor_scalar_add(st[:, s, 14:15], st[:, s, 13:14], EPS), R=[Bst[s]], W=[Bst[s]])
            act.op(lambda e: e.sqrt(st[:, s, 15:16], st[:, s, 14:15]), R=[Bst[s]], W=[Bst[s]])
            dve.op(lambda e: e.reciprocal(st[:, s, 16:17], st[:, s, 15:16]), R=[Bst[s]], W=[Bst[s]])
            dve.op(lambda e: e.tensor_scalar(tmp[:, s, :], tmp[:, s, :], st[:, s, 12:13], st[:, s, 16:17],
                                             ALU.subtract, ALU.mult), R=[Btmp[s], Bst[s]], W=[Btmp[s]])
            pool.op(lambda e: e.tensor_mul(tmp[:, s, :], tmp[:, s, :], gb[:, 0, :]), R=[Btmp[s], Bgb], W=[Btmp[s]])
            pool.op(lambda e: e.tensor_add(X[:, t, :], tmp[:, s, :], gb[:, 1, :]), R=[Btmp[s], Bgb], W=[BX[t]])

    o_ctr = [0]

    def attention_core(kind, qt, Bqt, kt, Bkt, vt, Bvt, blocks_for_ls, ps, Bps_, sb, Bsb, sink_ap=None, Bsink=None):
        S_ps, O_ps2, T_ps, R_ps, Bc_ps = ps["S"], ps["O"], ps["T"], ps["R"], ps["B"]
        BS, BO2, BT, BR, BB = Bps_["S"], Bps_["O"], Bps_["T"], Bps_["R"], Bps_["B"]
        at, Bat = sb["at"], Bsb["at"]
        ot, Bot = sb["ot"], Bsb["ot"]
        el, ll = sb["el"], sb["ll"]
        Bel, Bll = Bsb["el"], Bsb["ll"]
        rs, Brs = sb["rs"], Bsb["rs"]
        for ls in range(4):
            o_ctr[0] += 1
            O_ps = O_ps2[:, o_ctr[0] % 2, :]
            BO = BO2[o_ctr[0] % 2]
            qs = qt[:, ls * 512:(ls + 1) * 512]
            blks = blocks_for_ls(ls)
            n = len(blks)
            if kind == "sb":
                blks = blks[::-1]

            sm_slots = [(S_ps[:, 0, :], BS[0]), (S_ps[:, 1, :], BS[1]), (T_ps[:, 0, :], BT[0]), (T_ps[:, 1, :], BT[1])]

            def scores(i, slot, close):
                kc, vb, m_ap, Bm = blks[i]
                dst, Bd = slot
                ksl = kt[:, kc:kc + 128]
                last = close and (m_ap is None)
                pe.op(lambda e: e.matmul(dst, ksl, qs, start=True, stop=last),
                      R=[Bkt, Bqt], W=[Bd], signal=last)
                if m_ap is not None:
                    pe.op(lambda e: e.matmul(dst, ident_b[:], m_ap, start=False, stop=close),
                          R=[Bm, Bc], W=[Bd], signal=close)

            def pv(i, nslot):
                kc, vb, m_ap, Bm = blks[i]
                s = i % nslot
                pe.op(lambda e: e.matmul(O_ps[0:65, :], vt[:, vb, :], at[:, s, :], start=(i == 0), stop=(i == n - 1)),
                      R=[Bvt, Bat[s]], W=[BO], signal=True)

            if kind == "sm":
                LA = 3
                for i in range(min(LA, n)):
                    scores(i, sm_slots[i % 4], True)
                for i in range(n):
                    s = i % 4
                    if i + LA < n:
                        scores(i + LA, sm_slots[(i + LA) % 4], True)
                    act.op(lambda e: e.activation(at[:, s, :], sm_slots[s][0], AF.Exp), R=[sm_slots[s][1]], W=[Bat[s]])
                    if i >= 1:
                        pv(i - 1, 4)
                pv(n - 1, 4)
            else:
                def stage_b(i):
                    s = i % 2
                    act.op(lambda e: e.activation(el[:, s, :], S_ps[:, s, :], AF.Exp), R=[BS[s]], W=[Bel[s]])
                    act.op(lambda e: e.activation(ll[:, s, :], el[:, s, :], AF.Ln, bias=1.0), R=[Bel[s]], W=[Bll[s]])
                scores(0, (S_ps[:, 0, :], BS[0]), True)
                stage_b(0)
                for i in range(n):
                    s = i % 2
                    if i + 1 < n:
                        scores(i + 1, (S_ps[:, (i + 1) % 2, :], BS[(i + 1) % 2]), True)
                        stage_b(i + 1)
                    scores(i, (T_ps[:, s, :], BT[s]), False)
                    pe.op(lambda e: e.matmul(T_ps[:, s, :], triu[:], ll[:, s, :], start=False, stop=True),
                          R=[Bll[s], Bc], W=[BT[s]])
                    if i < n - 1:
                        pe.op(lambda e: e.matmul(R_ps[:, :], negones[:], ll[:, s, :], start=True, stop=True),
                              R=[Bll[s], Bc], W=[BR], signal=True)
                    if i == 0:
                        act.op(lambda e: e.activation(at[:, s, :], T_ps[:, s, :], AF.Exp), R=[BT[s]], W=[Bat[s]])
                    else:
                        dve.op(lambda e: e.tensor_tensor(el[:, s, :], T_ps[:, s, :], rs[:, :], ALU.add),
                               R=[BT[s], Brs], W=[Bel[s]])
                        act.op(lambda e: e.activation(at[:, s, :], el[:, s, :], AF.Exp), R=[Bel[s]], W=[Bat[s]])
                    if i < n - 1:
                        if i == 0:
                            dve.op(lambda e: e.tensor_copy(rs[:, :], R_ps[:, :]), R=[BR], W=[Brs])
                        else:
                            dve.op(lambda e: e.tensor_tensor(rs[:, :], rs[:, :], R_ps[:, :], ALU.add), R=[BR, Brs], W=[Brs])
                    if i >= 1:
                        pv(i - 1, 2)
                pv(n - 1, 2)
            osl = ot[0:64, ls * 512:(ls + 1) * 512]
            if kind == "sm":
                rd = sb["rd"]
                Brd = Bsb["rd"]
                if sink_ap is not None:
                    dve.op(lambda e: e.tensor_scalar_add(rd[64:65, :], O_ps[64:65, :], sink_ap), R=[BO, Bsink], W=[Brd])
                    dve.op(lambda e: e.reciprocal(rd[64:65, :], rd[64:65, :]), R=[Brd], W=[Brd])
                else:
                    dve.op(lambda e: e.reciprocal(rd[64:65, :], O_ps[64:65, :]), R=[BO], W=[Brd])
                pe.op(lambda e: e.matmul(Bc_ps[0:64, :], ones_f[64:65, 0:64], rd[64:65, :], start=True, stop=True),
                      R=[Brd, Bc], W=[BB])
                act.op(lambda e: e.copy(sb["bc"][0:64, :], Bc_ps[0:64, :]), R=[BB], W=[Bsb["bc"]])
                dve.op(lambda e: e.tensor_tensor(osl, O_ps[0:64, :], sb["bc"][0:64, :], ALU.mult),
                       R=[BO, Bsb["bc"]], W=[Bot])
            else:
                dve.op(lambda e: e.tensor_copy(osl, O_ps[0:64, :]), R=[BO], W=[Bot])

    from contextlib import ExitStack

    uid = [0]

    def SB(es, name, shape, dt):
        uid[0] += 1
        return es.enter_context(nc.sbuf_tensor("%s_%d" % (name, uid[0]), list(shape), dt))

    def PS(es, name, shape, dt):
        uid[0] += 1
        return es.enter_context(nc.psum_tensor("%s_%d" % (name, uid[0]), list(shape), dt))

    def split3(es, tag, src, Bsrc, dst3, Bdst, width, neg_dst=None):
        r = SB(es, "sp_r" + tag, [8, width], F32)
        Br = Buf("spr")
        dve.op(lambda e: e.tensor_copy(dst3[:, 0, :], src), R=[Bsrc], W=[Bdst])
        dve.op(lambda e: e.tensor_tensor(r[:], src, dst3[:, 0, :], ALU.subtract), R=[Bsrc, Bdst], W=[Br])
        dve.op(lambda e: e.tensor_copy(dst3[:, 1, :], r[:]), R=[Br], W=[Bdst])
        dve.op(lambda e: e.tensor_tensor(r[:], r[:], dst3[:, 1, :], ALU.subtract), R=[Br, Bdst], W=[Br])
        dve.op(lambda e: e.tensor_copy(dst3[:, 2, :], r[:]), R=[Br], W=[Bdst])
        if neg_dst is not None:
            for j in range(3):
                dve.op(lambda e: e.tensor_scalar_mul(neg_dst[:, j, :], dst3[:, j, :], -1.0), R=[Bdst], W=[Bdst])

    def fp32_proj8(es, w_ap, out_sb, Bout):
        wsb = SB(es, "w8", [128, 8, 8], F32)
        xtf = SB(es, "xtf", [128, 2, D], F32)
        XTp = PS(es, "XTp", [128, 2, 1024], F32)
        FAp = PS(es, "FAp", [128, NT, 8], F32)
        Bw, Bxtf, BXTp, BFAp = Buf("w8"), bufs("xtf", 2), bufs("XTp", 2), Buf("FAp")
        sp.dma(wsb[:], w_ap.rearrange("(k p) e -> p k e", p=128), Bw)
        for t in range(NT):
            s = t % 2
            for k in range(8):
                pe.op(lambda e: e.transpose(XTp[:, s, k * 128:(k + 1) * 128], X[:, t, k * 128:(k + 1) * 128], ident_f[:]),
                      R=[BX[t], Bc], W=[BXTp[s]], signal=(k == 7))
            dve.op(lambda e: e.tensor_copy(xtf[:, s, 0:512], XTp[:, s, 0:512]), R=[BXTp[s]], W=[Bxtf[s]])
            act.op(lambda e: e.copy(xtf[:, s, 512:1024], XTp[:, s, 512:1024]), R=[BXTp[s]], W=[Bxtf[s]])
            for k in range(8):
                pe.op(lambda e: e.matmul(FAp[:, t, :], xtf[:, s, k * 128:(k + 1) * 128], wsb[:, k, :], start=(k == 0), stop=(k == 7)),
                      R=[Bxtf[s], Bw], W=[BFAp], signal=(k == 7))
        dve.op(lambda e: e.tensor_copy(out_sb[:, :, :], FAp[:, :, :]), R=[BFAp], W=[Bout])

    def even_proj(j):
        with ExitStack() as es:
            bfb = SB(es, "bfb", [128, 8], F32)
            fa = SB(es, "fa", [128, NT, 8], F32)
            lfT = SB(es, "lfT", [8, T], F32)
            lf3 = SB(es, "lf3", [8, 3, T], BF16)
            P = PS(es, "Pl", [128, 2, 512], F32)
            Bwfb, Bbfb, Bjunk, Bfa, BlfT, Blf3 = Buf("wfb"), Buf("bfb"), bufs("junk", 2), Buf("fa"), Buf("lfT"), Buf("lf3")
            Bjunkb = Buf("junkb")
            BP = bufs("Pl", 2)
            sp.dma(bfb[:], ab_bf_in[j].partition_broadcast(128), Bbfb)
            fp32_proj8(es, ab_w_in[j][:, 1536:1544], fa, Bfa)
            for t in range(NT):
                dve.op(lambda e: e.tensor_tensor(fa[:, t, :], fa[:, t, :], bfb[:], ALU.add), R=[Bfa, Bbfb], W=[Bfa])
            faf = fa[:].rearrange("p t e -> p (t e)")
            act.op(lambda e: e.activation(faf, faf, AF.Exp, scale=-1.0), R=[Bfa], W=[Bfa])
            act.op(lambda e: e.activation(faf, faf, AF.Ln, bias=1.0), R=[Bfa], W=[Bfa])
            for g in range(4):
                for tt in range(4):
                    t = g * 4 + tt
                    pe.op(lambda e: e.transpose(P[0:8, g % 2, tt * 128:(tt + 1) * 128], fa[:, t, :], ident_f[:]),
                          R=[Bfa, Bc], W=[BP[g % 2]], signal=(tt == 3))
                act.op(lambda e: e.mul(lfT[:, g * 512:(g + 1) * 512], P[0:8, g % 2, :], -1.0), R=[BP[g % 2]], W=[BlfT])
            split3(es, "a", lfT[:], BlfT, lf3, Blf3, T)
            for jj in range(3):
                sp.dma(exm(2048 + jj * 8, 2048 + jj * 8 + 8), lf3[:, jj, :], B_exm, R=[Blf3])
            K.barrier()
        if os.environ.get("DBG_SUB") == "1":
            return
        with ExitStack() as es:
            wq = SB(es, "wq", [128, 8, 1024], BF16)
            wk = SB(es, "wk", [128, 8, 1024], BF16)
            wv = SB(es, "wv", [128, 8, 1024], BF16)
            stg = SB(es, "stg", [128, 2, T], BF16)
            vstg = SB(es, "vstg", [128, 2, 1024], BF16)
            P = PS(es, "P", [128, 2, 512], F32)
            Vp = PS(es, "Vp", [128, 2, 1024], F32)
            Bwq, Bwk, Bwv, Bwfb, Bbfb = Buf("wq"), Buf("wk"), Buf("wv"), Buf("wfb"), Buf("bfb")
            Bstg, Bvstg, BP, BVp = bufs("stg", 2), bufs("vstg", 2), bufs("P", 2), bufs("Vp", 2)
            Bjunk, Bfa, BlfT, Blf3 = Buf("junk"), Buf("fa"), Buf("lfT"), Buf("lf3")
            w = ab_w_in[j]
            for (dst, Bd, c0, c1) in ((wq, Bwq, 0, 1544), (wk, Bwk, 512, 2056), (wv, Bwv, 1024, 2568)):
                pool.dma(dst[:, :, 0:512], w[:, c0:c0 + 512].rearrange("(k p) c -> p k c", p=128), Bd)
                pool.dma(dst[:, :, 512:1024], w[:, c1:c1 + 512].rearrange("(k p) c -> p k c", p=128), Bd)
            u = 0
            for which, wt, Bw in (("q", wq, Bwq), ("k", wk, Bwk)):
                for hp in range(8):
                    s = u % 2
                    for tg in range(4):
                        ps_ = (u * 4 + tg) % 2
                        for k in range(8):
                            pe.op(lambda e: e.matmul(P[:, ps_, :], wt[:, k, hp * 128:(hp + 1) * 128],
                                                     XT[:, k, tg * 512:(tg + 1) * 512], start=(k == 0), stop=(k == 7)),
                                  R=[Bw] + BXT[tg * 4:(tg + 1) * 4], W=[BP[ps_]], signal=(k == 7))
                        sc = 0.125 if which == "q" else 1.0
                        act.op(lambda e: e.mul(stg[:, s, tg * 512:(tg + 1) * 512], P[:, ps_, :], sc),
                               R=[BP[ps_]], W=[Bstg[s]])
                    if which == "q":
                        sp.dma(q_scr.ap()[2 * hp:2 * hp + 2].rearrange("two d t -> (two d) t"), stg[:, s, :], B_qscr, R=[Bstg[s]])
                    else:
                        sp.dma(exm(hp * 128, (hp + 1) * 128), stg[:, s, :], B_exm, R=[Bstg[s]])
                    u += 1
            for t in range(NT):
                s = t % 2
                for c in range(2):
                    for k in range(8):
                        pe.op(lambda e: e.matmul(Vp[:, s, c * 512:(c + 1) * 512], XT[:, k, t * 128:(t + 1) * 128],
                                                 wv[:, k, c * 512:(c + 1) * 512], start=(k == 0), stop=(k == 7)),
                              R=[Bwv, BXT[t]], W=[BVp[s]], signal=(k == 7 and c == 1))
                act.op(lambda e: e.copy(vstg[:, s, :], Vp[:, s, :]), R=[BVp[s]], W=[Bvstg[s]])
                sp.dma(tokview(exm(1024 + t * 64, 1024 + (t + 1) * 64)), vstg[:, s, :], B_exm, R=[Bvstg[s]])
            if os.environ.get("DBG_SUB") == "2":
                K.barrier()
                return
            allgather(ex_mine_c, ex_all_c, B_exm, B_exa)
            K.barrier()
        if os.environ.get("DBG_SUB") == "3":
            return
        with ExitStack() as es:
            l3 = SB(es, "l3", [8, 3, S], BF16)
            lfa = SB(es, "lfa", [8, S], F32)
            onesr = SB(es, "onesr", [8, S], BF16)
            cT = SB(es, "cT", [8, S], F32)
            cown = SB(es, "cown", [8, T], F32)
            cq3 = SB(es, "cq3", [8, 3, T], BF16)
            osl = SB(es, "osl", [8, 8], F32)
            Bl3, Blfa, Bones, BcT, Bcown, Bcq3, Bosl = (Buf("l3"), Buf("lfa"), Buf("onesr"), Buf("cT"),
                                                         Buf("cown"), Buf("cq3"), Buf("osl"))
            sp.dma(osl[:], osel_in[0].partition_broadcast(8), Bosl)
            for gs in range(8):
                r, li = OWNER[gs], LIDX[gs]
                for jj in range(3):
                    sp.dma(l3[:, jj, gs * 512:(gs + 1) * 512], exa(r, 2048 + jj * 8, 2048 + jj * 8 + 8)[:, li * 512:(li + 1) * 512],
                           Bl3, R=[B_exa])
            dve.op(lambda e: e.memset(onesr[:], 1.0), W=[Bones])
            dve.op(lambda e: e.tensor_tensor(lfa[:], l3[:, 0, :], l3[:, 1, :], ALU.add), R=[Bl3], W=[Blfa])
            dve.op(lambda e: e.tensor_tensor(lfa[:], lfa[:], l3[:, 2, :], ALU.add), R=[Bl3, Blfa], W=[Blfa])
            dve.op(lambda e: e.tensor_tensor_scan(cT[:], onesr[:], lfa[:], 0.0, ALU.mult, ALU.add), R=[Bones, Blfa], W=[BcT])
            split3(es, "b", cT[:], BcT, l3, Bl3, S)
            sp.dma(c_scr.ap()[:, 0:3, :], l3[:], B_cscr, R=[Bl3])
            for jj in range(3):
                dve.op(lambda e: e.tensor_scalar_mul(l3[:, jj, :], l3[:, jj, :], -1.0), R=[Bl3], W=[Bl3])
            sp.dma(c_scr.ap()[:, 3:6, :], l3[:], B_cscr, R=[Bl3])
            for ls in range(4):
                a0 = GSMIN[ls] * 512
                dve.op(lambda e: e.tensor_scalar_mul(cown[:, ls * 512:(ls + 1) * 512], cT[:, a0:a0 + 512],
                                                     osl[:, 2 * ls:2 * ls + 1]), R=[BcT, Bosl], W=[Bcown])
                dve.op(lambda e: e.scalar_tensor_tensor(cown[:, ls * 512:(ls + 1) * 512], cT[:, a0 + 512:a0 + 1024],
                                                        osl[:, 2 * ls + 1:2 * ls + 2], cown[:, ls * 512:(ls + 1) * 512],
                                                        ALU.mult, ALU.add), R=[BcT, Bosl, Bcown], W=[Bcown])
            split3(es, "c", cown[:], Bcown, cq3, Bcq3, T)
            sp.dma(cq_scr.ap(), cq3[:], B_cqscr, R=[Bcq3])
            K.barrier()

    def attn_alloc(es, nkcols, nvblk):
        d = {}
        d["qt"] = SB(es, "qt", [128, 2, T], BF16)
        d["kt"] = SB(es, "kt", [128, 2, nkcols], BF16)
        d["vt"] = SB(es, "vt", [128, 2, nvblk, 65], BF16)
        sbt = {"at": SB(es, "at", [128, 4, 512], BF16), "el": SB(es, "el", [128, 2, 512], F32),
               "ll": SB(es, "ll", [128, 2, 512], BF16), "rs": SB(es, "rs", [128, 512], F32),
               "ot": SB(es, "ot", [64, 2, T], BF16), "rd": SB(es, "rd", [128, 512], F32),
               "bc": SB(es, "bc", [64, 512], F32)}
        Bsb = {"at": bufs("at", 4), "el": bufs("el", 2), "ll": bufs("ll", 2), "rs": Buf("rs"),
               "ot": bufs("ot", 2), "rd": Buf("rd"), "bc": Buf("bc")}
        ps = {"S": PS(es, "S", [128, 2, 512], F32), "T": PS(es, "T", [128, 2, 512], F32),
              "R": PS(es, "R", [128, 512], F32), "O": PS(es, "O", [128, 2, 512], F32), "B": PS(es, "B", [128, 512], F32)}
        Bps = {"S": bufs("S", 2), "T": bufs("T", 2), "R": Buf("R"), "O": bufs("O", 2), "B": Buf("B")}
        d["Bqt"], d["Bkt"], d["Bvt"] = bufs("qt", 2), bufs("kt", 2), bufs("vt", 2)
        for s in range(2):
            dve.op(lambda e: e.memset(d["qt"][:, s, :], 0.0), W=[d["Bqt"][s]])
            pool.op(lambda e: e.memset(d["kt"][:, s, :], 0.0), W=[d["Bkt"][s]])
            dve.op(lambda e: e.memset(d["vt"][:, s, :, :], 1.0), W=[d["Bvt"][s]])
        return d, sbt, Bsb, ps, Bps

    def even_attn(j):
        with ExitStack() as es:
            d, sbt, Bsb, ps, Bps = attn_alloc(es, S, 32)
            qt, kt, vt = d["qt"], d["kt"], d["vt"]
            mle = SB(es, "mle", [128, 2, 8, 512], BF16)
            mlt = SB(es, "mlt", [128, 2, 8, 512], BF16)
            Bmle, Bmlt = Buf("mle"), Buf("mlt")
            for o in range(2):
                pool.dma(mle[:, o], mle_in[o].rearrange("r p q -> p r q"), Bmle)
                pool.dma(mlt[:, o], mlt_in[o].rearrange("r p q -> p r q"), Bmlt)
            for s in range(2):
                dve.op(lambda e: e.memset(qt[64:70, s, :], 1.0), W=[d["Bqt"][s]])
                dve.op(lambda e: e.memset(kt[64:70, s, :], 1.0), W=[d["Bkt"][s]])
            for h in range(16):
                s = h % 2
                fox = h < 8
                Bq, Bk, Bv = d["Bqt"][s], d["Bkt"][s], d["Bvt"][s]
                if h == 8 or h == 9:
                    dve.op(lambda e: e.memset(qt[64:70, s, :], 0.0), W=[Bq])
                sp.dma(qt[0:64, s, :], q_scr.ap()[h], Bq, R=[B_qscr])
                if fox:
                    sp.dma(qt[64:67, s, :], cq_scr.ap()[h], Bq, R=[B_cqscr])
                    sp.dma(kt[67:70, s, :], c_scr.ap()[h, 3:6, :], Bk, R=[B_cscr])
                for gs in range(8):
                    r, li = OWNER[gs], LIDX[gs]
                    sp.dma(kt[0:64, s, gs * 512:(gs + 1) * 512],
                           exa(r, h * 64, (h + 1) * 64)[:, li * 512:(li + 1) * 512], Bk, R=[B_exa])
                    vsec = tokview(exa(r, 1024 + li * 256, 1024 + (li + 1) * 256))
                    sp.dma(vt[:, s, gs * 4:(gs + 1) * 4, 0:64],
                           vsec[:, h * 64:(h + 1) * 64].rearrange("(j p) d -> p j d", p=128),
                           Bv, R=[B_exa])
                msk, Bm = (mle, Bmle) if fox else (mlt, Bmlt)

                def blocks(ls):
                    nkb = 4 * (GSMIN[ls] + 2)
                    res = []
                    for kb in range(nkb):
                        rr = kb - 4 * GSMIN[ls]
                        m_ap = msk[:, ls % 2, rr, :] if rr >= 0 else None
                        res.append((kb * 128, kb, m_ap, Bm))
                    return res
                attention_core("sm" if fox else "sb", qt[:, s, :], Bq, kt[:, s, :], Bk, vt[:, s], Bv, blocks,
                               ps, Bps, {**sbt, "ot": sbt["ot"][:, s, :]}, {**Bsb, "ot": Bsb["ot"][s]})
                sp.dma(ot_scr.ap()[h], sbt["ot"][:, s, :], B_otscr, R=[Bsb["ot"][s]])
            K.barrier()

    def out_proj_ln(w_out_ap, ln_idx):
        with ExitStack() as es:
            wo = SB(es, "wo", [128, 8, D], BF16)
            otl = SB(es, "otl", [128, 2, 8, 512], BF16)
            gb = SB(es, "gb", [128, 2, D], F32)
            tmp = SB(es, "tmp", [128, 2, D], F32)
            st = SB(es, "st", [128, 2, 32], F32)
            xb = SB(es, "xb", [128, 2, D], BF16)
            y_ps = PS(es, "yps", [128, 2, 1024], F32)
            tr_ps = PS(es, "trps", [128, 2, 1024], BF16)
            Bwo, Bgb = Buf("wo"), Buf("gb")
            Botl = bufs("otl", 2)
            Btmp, Bst, Bxb, By, Btr = bufs("tmp", 2), bufs("st", 2), bufs("xb", 2), bufs("y", 2), bufs("tr", 2)
            pool.dma(wo[:], w_out_ap.rearrange("(hp p) n -> p hp n", p=128), Bwo)

            def get_y(t, s):
                tg, tt = t // 4, t % 4
                so = tg % 2
                if tt == 0:
                    sp.dma(otl[:, so], ot_scr.ap()[:, :, tg * 512:(tg + 1) * 512].rearrange("(hp two) d t -> (two d) hp t", two=2),
                           Botl[so], R=[B_otscr])
                for c in range(2):
                    for h in range(8):
                        pe.op(lambda e: e.matmul(y_ps[:, s, c * 512:(c + 1) * 512], otl[:, so, h, tt * 128:(tt + 1) * 128],
                                                 wo[:, h, c * 512:(c + 1) * 512], start=(h == 0), stop=(h == 7)),
                              R=[Botl[so], Bwo], W=[By[s]], signal=(h == 7 and c == 1))
                dve.op(lambda e: e.scalar_tensor_tensor(tmp[:, s, :], X[:, t, :], ALPHA, y_ps[:, s, :], ALU.mult, ALU.add),
                       R=[BX[t], By[s]], W=[Btmp[s]])
            layer_norm_tiles(ln_idx, get_y, gb, Bgb, tmp, Btmp, st, Bst)
            make_xt(tr_ps, Btr, xb, Bxb)
            K.barrier()

    def ffn_phase(experts, d_ff, comb=None, Bcomb=None):
        with ExitStack() as es:
            wgu = SB(es, "wgu", [128, 2, 2, 8, 512], BF16)
            wdn = SB(es, "wdn", [128, 2, 4, D], BF16)
            actT = SB(es, "actT", [128, 2, 4, T], BF16)
            sg = SB(es, "sg", [128, 2, 512], F32)
            GU = PS(es, "GU", [128, 4, 512], F32)
            Y = PS(es, "Y", [128, 2, 1024], F32)
            Bwgu, Bwdn, BactT, Bsg, BGU, BY = bufs("wgu", 2), bufs("wdn", 2), bufs("actT", 2), bufs("sg", 2), bufs("GU", 2), bufs("Y", 2)
            for t in range(NT):
                dve.op(lambda e: e.tensor_scalar_mul(X[:, t, :], X[:, t, :], ALPHA), R=[BX[t]], W=[BX[t]])
            nch = d_ff // 128
            groups = [(g0, min(4, nch - g0)) for g0 in range(0, nch, 4)]
            gi = 0
            cnt = 0
            for ei, (wg_ap, wd_ap) in enumerate(experts):
                for (g0, gn) in groups:
                    s = gi % 2
                    c0 = g0 * 128
                    pool.dma(wgu[:, s, 0, :, 0:gn * 128],
                             wg_ap[:, c0:c0 + gn * 128].rearrange("(k p) n -> p k n", p=128), Bwgu[s])
                    pool.dma(wgu[:, s, 1, :, 0:gn * 128],
                             wg_ap[:, d_ff + c0:d_ff + c0 + gn * 128].rearrange("(k p) n -> p k n", p=128), Bwgu[s])
                    pool.dma(wdn[:, s, 0:gn, :], wd_ap[c0:c0 + gn * 128, :].rearrange("(c p) n -> p c n", p=128), Bwdn[s])
                    for c in range(gn):
                        for tg in range(4):
                            ps_ = cnt % 2
                            cnt += 1
                            for half in range(2):
                                for k in range(8):
                                    pe.op(lambda e: e.matmul(GU[:, ps_ * 2 + half, :], wgu[:, s, half, k, c * 128:(c + 1) * 128],
                                                             XT[:, k, tg * 512:(tg + 1) * 512], start=(k == 0), stop=(k == 7)),
                                          R=[Bwgu[s]] + BXT[tg * 4:(tg + 1) * 4], W=[BGU[ps_]], signal=(k == 7 and half == 1))
                            act.op(lambda e: e.activation(sg[:, ps_, :], GU[:, ps_ * 2, :], AF.Silu), R=[BGU[ps_]], W=[Bsg[ps_]])
                            dve.op(lambda e: e.tensor_tensor(actT[:, s, c, tg * 512:(tg + 1) * 512], sg[:, ps_, :],
                                                             GU[:, ps_ * 2 + 1, :], ALU.mult),
                                   R=[Bsg[ps_], BGU[ps_]], W=[BactT[s]])
                    for t in range(NT):
                        ys = t % 2
                        for cc in range(2):
                            for c in range(gn):
                                pe.op(lambda e: e.matmul(Y[:, ys, cc * 512:(cc + 1) * 512], actT[:, s, c, t * 128:(t + 1) * 128],
                                                         wdn[:, s, c, cc * 512:(cc + 1) * 512], start=(c == 0), stop=(c == gn - 1)),
                                      R=[BactT[s], Bwdn[s]], W=[BY[ys]], signal=(c == gn - 1 and cc == 1))
                        if comb is None:
                            dve.op(lambda e: e.tensor_tensor(X[:, t, :], X[:, t, :], Y[:, ys, :], ALU.add),
                                   R=[BX[t], BY[ys]], W=[BX[t]])
                        else:
                            dve.op(lambda e: e.scalar_tensor_tensor(X[:, t, :], Y[:, ys, :], comb[:, t, ei:ei + 1], X[:, t, :],
                                                                    ALU.mult, ALU.add),
                                   R=[BX[t], BY[ys], Bcomb], W=[BX[t]])
                    gi += 1
            K.barrier()

    def ln2_ple(i):
        with ExitStack() as es:
            gb = SB(es, "gb2", [128, 2, D], F32)
            tmp = SB(es, "tmp2", [128, 2, D], F32)
            st = SB(es, "st2", [128, 2, 32], F32)
            xb = SB(es, "xb2", [128, 2, D], BF16)
            wg = SB(es, "wg2", [128, 8, D], BF16)
            wp = SB(es, "wp2", [128, 2, D], BF16)
            pb = SB(es, "pb2", [128, 2, 256], BF16)
            pT = SB(es, "pT2", [128, 2, 2, 128], BF16)
            sig = SB(es, "sig2", [128, 2, D], F32)
            tr_ps = PS(es, "trps2", [128, 2, 1024], BF16)
            G = PS(es, "G2", [128, 1024], F32)
            PP = PS(es, "PP2", [128, 1024], F32)
            ptr = PS(es, "ptr2", [128, 1024], BF16)
            Bgb, Bwg, Bwp = Buf("gb"), Buf("wg"), Buf("wp")
            Btmp, Bst, Bxb, Btr, Bpb, BpT, Bsig = (bufs("tmp", 2), bufs("st", 2), bufs("xb", 2), bufs("tr", 2), bufs("pb", 2),
                                                   bufs("pT", 2), bufs("sig", 2))
            BG, BPP, Bptr = Buf("G"), Buf("PP"), Buf("ptr")
            pool.dma(wg[:], ple_g[i].rearrange("(k p) n -> p k n", p=128), Bwg)
            pool.dma(wp[:], ple_p[i].rearrange("(k p) n -> p k n", p=128), Bwp)

            def get_y(t, s):
                dve.op(lambda e: e.tensor_copy(tmp[:, s, :], X[:, t, :]), R=[BX[t]], W=[Btmp[s]])
            layer_norm_tiles(2 * i + 1, get_y, gb, Bgb, tmp, Btmp, st, Bst)
            make_xt(tr_ps, Btr, xb, Bxb)
            for t in range(NT):
                s = t % 2
                pool.dma(pb[:, s, :], p_in[i, t * 128:(t + 1) * 128, :], Bpb[s])
                for k in range(2):
                    pe.op(lambda e: e.transpose(ptr[:, k * 128:(k + 1) * 128], pb[:, s, k * 128:(k + 1) * 128], ident_b[:]),
                          R=[Bpb[s], Bc], W=[Bptr], signal=(k == 1))
                act.op(lambda e: e.copy(pT[:, s, :, :], ptr[:, 0:256].rearrange("p (k n) -> p k n", k=2)), R=[Bptr], W=[BpT[s]])
                for c in range(2):
                    for k in range(8):
                        pe.op(lambda e: e.matmul(G[:, c * 512:(c + 1) * 512], XT[:, k, t * 128:(t + 1) * 128],
                                                 wg[:, k, c * 512:(c + 1) * 512], start=(k == 0), stop=(k == 7)),
                              R=[BXT[t], Bwg], W=[BG], signal=(k == 7 and c == 1))
                for c in range(2):
                    for k in range(2):
                        pe.op(lambda e: e.matmul(PP[:, c * 512:(c + 1) * 512], pT[:, s, k, :],
                                                 wp[:, k, c * 512:(c + 1) * 512], start=(k == 0), stop=(k == 1)),
                              R=[BpT[s], Bwp], W=[BPP], signal=(k == 1 and c == 1))
                act.op(lambda e: e.activation(sig[:, s, :], G[:, :], AF.Sigmoid), R=[BG], W=[Bsig[s]])
                dve.op(lambda e: e.tensor_tensor(sig[:, s, :], sig[:, s, :], PP[:, :], ALU.mult), R=[Bsig[s], BPP], W=[Bsig[s]])
                dve.op(lambda e: e.tensor_tensor(X[:, t, :], X[:, t, :], sig[:, s, :], ALU.add), R=[BX[t], Bsig[s]], W=[BX[t]])
            make_xt(tr_ps, Btr, xb, Bxb)
            K.barrier()

    def odd_proj_attn(j):
        with ExitStack() as es:
            wqkv = SB(es, "wqkv", [128, 8, 1280], BF16)
            cst = SB(es, "cst", [128, 2, NT, 8], F32)
            qkv = SB(es, "qkv", [128, 2, 1280], F32)
            rt = SB(es, "rt", [128, 4, 18, 8], F32)
            qb = SB(es, "qb", [128, 2, 1280], BF16)
            qstg = SB(es, "qstg", [64, 2, 8, 128], BF16)
            QKV = PS(es, "QKV", [128, 3, 512], F32)
            TR = PS(es, "TR", [128, 2, 1024], BF16)
            Bw, Bcs, Brt = Buf("wqkv"), Buf("cst"), Buf("rt")
            Bqkv, Bqb, Bqstg, BTR = bufs("qkv", 2), bufs("qb", 2), bufs("qstg", 2), bufs("TR", 2)
            BQKV = Buf("QKV")
            pool.dma(wqkv[:], c_w_qkv[j].rearrange("(k p) n -> p k n", p=128), Bw)
            sp.dma(cst[:, 0], cos_in.rearrange("(t p) e -> p t e", p=128), Bcs)
            sp.dma(cst[:, 1], sin_in.rearrange("(t p) e -> p t e", p=128), Bcs)
            vsec = ex2_mine.ap()[128:256, :].rearrange("r (a c) -> (r a) c", c=128)
            gcount = 0
            for t in range(NT):
                s = t % 2
                for c, (c0, c1) in enumerate(((0, 512), (512, 1024), (1024, 1280))):
                    for k in range(8):
                        pe.op(lambda e: e.matmul(QKV[:, c, 0:c1 - c0], XT[:, k, t * 128:(t + 1) * 128], wqkv[:, k, c0:c1],
                                                 start=(k == 0), stop=(k == 7)),
                              R=[BXT[t], Bw], W=[BQKV], signal=(k == 7 and c == 2))
                for c, (c0, c1) in enumerate(((0, 512), (512, 1024), (1024, 1280))):
                    act.op(lambda e: e.copy(qkv[:, s, c0:c1], QKV[:, c, 0:c1 - c0]), R=[BQKV], W=[Bqkv[s]])
                hv = qkv[:, s, 0:1152].rearrange("p (h d) -> p h d", d=64)
                x1, x2 = hv[:, :, 0:8], hv[:, :, 8:16]
                cosb = cst[:, 0, t:t + 1, :].to_broadcast([128, 18, 8])
                sinb = cst[:, 1, t:t + 1, :].to_broadcast([128, 18, 8])
                dve.op(lambda e: e.tensor_tensor(rt[:, 0], x1, cosb, ALU.mult), R=[Bqkv[s], Bcs], W=[Brt])
                dve.op(lambda e: e.tensor_tensor(rt[:, 1], x2, sinb, ALU.mult), R=[Bqkv[s], Bcs], W=[Brt])
                dve.op(lambda e: e.tensor_tensor(rt[:, 2], x2, cosb, ALU.mult), R=[Bqkv[s], Bcs], W=[Brt])
                dve.op(lambda e: e.tensor_tensor(rt[:, 3], x1, sinb, ALU.mult), R=[Bqkv[s], Bcs], W=[Brt])
                dve.op(lambda e: e.tensor_tensor(x1, rt[:, 0], rt[:, 1], ALU.subtract), R=[Brt], W=[Bqkv[s]])
                dve.op(lambda e: e.tensor_tensor(x2, rt[:, 2], rt[:, 3], ALU.add), R=[Brt], W=[Bqkv[s]])
                act.op(lambda e: e.mul(qb[:, s, 0:1024], qkv[:, s, 0:1024], 0.125), R=[Bqkv[s]], W=[Bqb[s]])
                act.op(lambda e: e.copy(qb[:, s, 1024:1280], qkv[:, s, 1024:1280]), R=[Bqkv[s]], W=[Bqb[s]])
                for g in range(3):
                    gs_ = gcount % 2
                    gcount += 1
                    nh = 8 if g < 2 else 2
                    for hh in range(nh):
                        h = g * 8 + hh
                        pe.op(lambda e: e.transpose(TR[0:64, gs_, hh * 128:(hh + 1) * 128], qb[:, s, h * 64:(h + 1) * 64], ident_b[:]),
                              R=[Bqb[s], Bc], W=[BTR[gs_]], signal=(hh == nh - 1))
                    dve.op(lambda e: e.tensor_copy(qstg[:, gs_, 0:nh, :],
                                                   TR[0:64, gs_, 0:nh * 128].rearrange("p (h n) -> p h n", n=128)),
                           R=[BTR[gs_]], W=[Bqstg[gs_]])
                    if g < 2:
                        sp.dma(q_scr.ap()[g * 8:(g + 1) * 8, :, t * 128:(t + 1) * 128].rearrange("h d n -> d h n"),
                               qstg[:, gs_, :, :], B_qscr, R=[Bqstg[gs_]])
                    else:
                        sp.dma(ex2_mine.ap()[0:128, t * 128:(t + 1) * 128].rearrange("(h d) n -> d h n", d=64),
                               qstg[:, gs_, 0:2, :], B_ex2m, R=[Bqstg[gs_]])
                sp.dma(vsec[t * 128:(t + 1) * 128, :], qb[:, s, 1152:1280], B_ex2m, R=[Bqb[s]])
            allgather(ex2_mine, ex2_all, B_ex2m, B_ex2a)
            K.barrier()
        with ExitStack() as es:
            d, sbt, Bsb, ps, Bps = attn_alloc(es, T + 7 * 128, NT + 7)
            qt, kt, vt = d["qt"], d["kt"], d["vt"]
            msw = SB(es, "msw", [128, 4, 6, 512], BF16)
            sk = SB(es, "sk", [128, 16], F32)
            Bmsw, Bsk = Buf("msw"), Buf("sk")
            for ls in range(4):
                pool.dma(msw[:, ls], msw_in[ls].rearrange("r p q -> p r q"), Bmsw)
            sp.dma(sk[:], c_sinks[j].partition_broadcast(128), Bsk)
            act.op(lambda e: e.activation(sk[:], sk[:], AF.Exp), R=[Bsk], W=[Bsk])
            cands = [c for l in SWA_CANDS for c in l]
            for kv in range(2):
                Bk, Bv = d["Bkt"][kv], d["Bvt"][kv]
                sp.dma(kt[0:64, kv, 0:T], ex2_mine.ap()[kv * 64:(kv + 1) * 64, :], Bk, R=[B_ex2m])
                sp.dma(vt[:, kv, 0:NT, 0:64], vsec_of(ex2_mine.ap(), 0)[:, kv * 64:(kv + 1) * 64].rearrange("(j p) d -> p j d", p=128),
                       Bv, R=[B_ex2m])
                for ci, (r, li) in enumerate(cands):
                    t0 = li * 512 + 384
                    sp.dma(kt[0:64, kv, T + ci * 128:T + (ci + 1) * 128],
                           ex2_all.ap()[r * 256 + kv * 64:r * 256 + (kv + 1) * 64, t0:t0 + 128], Bk, R=[B_ex2a])
                    sp.dma(vt[:, kv, NT + ci, 0:64], vsec_of(ex2_all.ap(), r * 256)[t0:t0 + 128, kv * 64:(kv + 1) * 64], Bv, R=[B_ex2a])
            cbase = [0, 1, 3, 5]
            for h in range(16):
                s = h % 2
                kv = h // 8
                Bq = d["Bqt"][s]
                sp.dma(qt[0:64, s, :], q_scr.ap()[h], Bq, R=[B_qscr])

                def blocks(ls):
                    res = []
                    for jj in range(4):
                        res.append(((4 * ls + jj) * 128, 4 * ls + jj, msw[:, ls, jj, :], Bmsw))
                    for cc in range(len(SWA_CANDS[ls])):
                        ci = cbase[ls] + cc
                        res.append((T + ci * 128, NT + ci, msw[:, ls, 4 + cc, :], Bmsw))
                    return res
                attention_core("sm", qt[:, s, :], Bq, kt[:, kv, :], d["Bkt"][kv], vt[:, kv], d["Bvt"][kv], blocks,
                               ps, Bps, {**sbt, "ot": sbt["ot"][:, s, :]}, {**Bsb, "ot": Bsb["ot"][s]},
                               sink_ap=sk[64:65, h:h + 1], Bsink=Bsk)
                sp.dma(ot_scr.ap()[h], sbt["ot"][:, s, :], B_otscr, R=[Bsb["ot"][s]])
            K.barrier()

    def vsec_of(ap, row0):
        return ap[row0 + 128:row0 + 256, :].rearrange("r (a c) -> (r a) c", c=128)

    def moe_phase(j):
        with ExitStack() as es:
            comb = SB(es, "comb", [128, NT, 8], F32)
            Bcomb = Buf("comb")
            with ExitStack() as es2:
                rbb = SB(es2, "rbb", [128, 8], F32)
                lg = SB(es2, "lg", [128, NT, 8], F32)
                mx = SB(es2, "mx", [128, NT, 8], F32)
                gg = SB(es2, "gg", [128, NT, 4], F32)
                mm = SB(es2, "mm", [128, NT, 8], F32)
                Brtb, Brbb, Blg, Bmx, Bgg, Bmm, Bjunk = Buf("rtb"), Buf("rbb"), Buf("lg"), Buf("mx"), Buf("gg"), Buf("mm"), bufs("junk", 2)
                Bjunkb = Buf("junkb")
                sp.dma(rbb[:], rb_in[j].partition_broadcast(128), Brbb)
                fp32_proj8(es2, router_in[j], lg, Blg)
                for t in range(NT):
                    dve.op(lambda e: e.tensor_tensor(lg[:, t, :], lg[:, t, :], rbb[:], ALU.add), R=[Blg, Brbb], W=[Blg])
                    dve.op(lambda e: e.max(mx[:, t, :], lg[:, t, :]), R=[Blg], W=[Bmx])
                    dve.op(lambda e: e.tensor_tensor(gg[:, t, 0:1], mx[:, t, 1:2], mx[:, t, 0:1], ALU.subtract), R=[Bmx], W=[Bgg])
                    act.op(lambda e: e.activation(gg[:, t, 1:2], gg[:, t, 0:1], AF.Sigmoid), R=[Bgg], W=[Bgg])
                    dve.op(lambda e: e.tensor_scalar(gg[:, t, 2:3], gg[:, t, 1:2], -1.0, 1.0, ALU.mult, ALU.add), R=[Bgg], W=[Bgg])
                    dve.op(lambda e: e.tensor_scalar(mm[:, t, :], lg[:, t, :], mx[:, t, 0:1], gg[:, t, 2:3], ALU.is_equal, ALU.mult),
                           R=[Blg, Bmx, Bgg], W=[Bmm])
                    dve.op(lambda e: e.tensor_scalar(comb[:, t, :], lg[:, t, :], mx[:, t, 1:2], gg[:, t, 1:2], ALU.is_equal, ALU.mult),
                           R=[Blg, Bmx, Bgg], W=[Bcomb])
                    dve.op(lambda e: e.tensor_tensor(comb[:, t, :], comb[:, t, :], mm[:, t, :], ALU.add), R=[Bmm, Bcomb], W=[Bcomb])
                K.barrier()
            ffn_phase([(moe_gu[j, e_], moe_dn[j, e_]) for e_ in range(8)], D_FFE, comb, Bcomb)

    def dump_x():
        for t in range(NT):
            sp.dma(out[t * 128:(t + 1) * 128, :], X[:, t, :], B_out, R=[BX[t]])
        for e in K.engs:
            e.wait(B_out.w)

    with ExitStack() as es0:
        xb0 = SB(es0, "xb0", [128, 2, D], BF16)
        tr0 = PS(es0, "tr0", [128, 2, 1024], BF16)
        make_xt(tr0, bufs("tr0", 2), xb0, bufs("xb0", 2))
        K.barrier()

    phases = []
    for i in range(layer0, layer0 + n_layers):
        j = i // 2
        if i % 2 == 0:
            phases += [lambda j=j: even_proj(j), lambda j=j: even_attn(j), lambda i=i, j=j: out_proj_ln(ab_w_out[j], 2 * i),
                       lambda j=j: ffn_phase([(ffn_gu[j], ffn_dn[j])], D_FF)]
        else:
            phases += [lambda j=j: odd_proj_attn(j), lambda i=i, j=j: out_proj_ln(c_w_out[j], 2 * i), lambda j=j: moe_phase(j)]
        phases.append(lambda i=i: ln2_ple(i))
    if dbg_stage is not None:
        phases = phases[:dbg_stage]
    for ph in phases:
        ph()
    dump_x()
    return nc


def _host_consts(parity):
    gs_own = OWN[parity]
    kk = np.arange(128)[:, None]
    qq = np.arange(512)[None, :]
    mle = np.zeros((2, 8, 128, 512), np.float32)
    mlt = np.zeros((2, 8, 128, 512), np.float32)
    for o in range(2):
        gs = gs_own[o]
        for r in range(8):
            kpos = (4 * GSMIN[o] + r) * 128 + kk
            qpos = gs * 512 + qq
            mle[o, r] = np.where(kpos <= qpos, 0.0, NEG)
            mlt[o, r] = np.where(kpos < qpos, 0.0, NEG)
    osel = np.zeros((1, 8), np.float32)
    for ls in range(4):
        o = gs_own[ls] - GSMIN[ls]
        osel[0, 2 * ls] = 1.0 - o
        osel[0, 2 * ls + 1] = float(o)
    msw = np.full((4, 6, 128, 512), NEG, np.float32)
    for ls in range(4):
        for jj in range(4):
            diff = qq - (jj * 128 + kk)
            msw[ls, jj] = np.where((diff >= 0) & (diff < 128), 0.0, NEG)
        prev = gs_own[ls] - 1
        for cc, (r, li) in enumerate(SWA_CANDS[ls]):
            real = prev >= 0 and OWNER[prev] == r and LIDX[prev] == li
            if real:
                diff = qq - (kk - 128)
                msw[ls, 4 + cc] = np.where((diff >= 0) & (diff < 128), 0.0, NEG)
    pos = np.concatenate([np.arange(g * 512, (g + 1) * 512) for g in gs_own]).astype(np.float32)
    inv = (500000.0 ** (-np.arange(8, dtype=np.float32) * 2.0 / 16.0)).astype(np.float32)
    ang = pos[:, None] * inv[None, :]
    cos = np.cos(ang).astype(np.float32)
    sin = np.sin(ang).astype(np.float32)
    return dict(mask_le=mle, mask_lt=mlt, osel=osel, mask_swa=msw, rope_cos=cos, rope_sin=sin)


_CACHE = {}


N_SPLIT = 1


def _run(inputs, n_layers=DEPTH, dbg_stage=None, trace=False, n_split=None):
    f = lambda a: np.ascontiguousarray(np.asarray(a, dtype=np.float32))
    x = f(inputs["x"])
    p = f(inputs["p"])
    if n_split is None:
        n_split = N_SPLIT if (n_layers == DEPTH and dbg_stage is None) else 1
    lng = np.empty((2 * DEPTH, D), np.float32)
    lnb = np.empty((2 * DEPTH, D), np.float32)
    lng[0::2] = f(inputs["ln_mix_g"]); lng[1::2] = f(inputs["ln_ffn_g"])
    lnb[0::2] = f(inputs["ln_mix_b"]); lnb[1::2] = f(inputs["ln_ffn_b"])
    ab_w_in = f(inputs["ab_w_in"])
    shared = dict(
        ln_g=lng, ln_b=lnb, ab_w_in=ab_w_in,
        ab_wfT=np.ascontiguousarray(ab_w_in[:, :, 1536:1544].transpose(0, 2, 1)),
        ab_b_f=f(inputs["ab_b_f"]), ab_w_out=f(inputs["ab_w_out"]), c_w_qkv=f(inputs["c_w_qkv"]),
        c_sinks=f(inputs["c_sinks"]), c_w_out=f(inputs["c_w_out"]), ffn_w_gate_up=f(inputs["ffn_w_gate_up"]),
        ffn_w_down=f(inputs["ffn_w_down"]),
        routerT=np.ascontiguousarray(f(inputs["router_w"]).transpose(0, 2, 1)), router_w=f(inputs["router_w"]),
        router_b=f(inputs["router_b"]),
        moe_w_gate_up=f(inputs["moe_w_gate_up"]), moe_w_down=f(inputs["moe_w_down"]),
        ple_w_gate=f(inputs["ple_w_gate"]), ple_w_proj=f(inputs["ple_w_proj"]),
        ident=np.eye(128, dtype=np.float32),
        triu_neg=np.where(np.arange(128)[:, None] >= np.arange(128)[None, :], -1.0, 0.0).astype(np.float32),
    )
    consts = [_host_consts(0), _host_consts(1)]
    idxs = [np.concatenate([np.arange(g * 512, (g + 1) * 512) for g in OWN[par]]) for par in range(2)]
    x_own = [np.ascontiguousarray(x[c // 2, idxs[c % 2]]) for c in range(8)]
    per = n_layers // n_split
    res = None
    for li in range(n_split):
        key = (per, dbg_stage, li * per)
        if key not in _CACHE:
            _CACHE[key] = build(per, dbg_stage, li * per)
        nc = _CACHE[key]
        in_maps = []
        for c in range(8):
            b, par = c // 2, c % 2
            m = dict(shared)
            m["x_own"] = x_own[c]
            m["p_own"] = np.ascontiguousarray(p[:, b, idxs[par]])
            m.update(consts[par])
            in_maps.append(m)
        res = run_bass_kernel_spmd(nc, in_maps, core_ids=list(range(8)), trace=trace)
        x_own = [np.ascontiguousarray(np.asarray(res.results[c]["out"], dtype=np.float32)) for c in range(8)]
    outp = np.empty((4, S, D), np.float32)
    for c in range(8):
        outp[c // 2, idxs[c % 2]] = x_own[c]
    return outp, res


def kernel(**inputs):
    outp, _ = _run(inputs)
    return outp
```
